# Optimizing a Trainium2 kernel written in Bass

```python
import jax, jax.numpy as jnp
from jax import lax
import numpy as np

D_MODEL = 2048
BATCH = 8
SEQ = 2048
DEPTH = 1

MIX_WIDTH = D_MODEL
HG_WIDTH = MIX_WIDTH // 2
HG_HEAD_DIM = 128
HG_HEADS = HG_WIDTH // HG_HEAD_DIM
HG_CHUNK = 64
NSA_WIDTH = MIX_WIDTH - HG_WIDTH
NSA_HEAD_DIM = 64
NSA_Q_HEADS = NSA_WIDTH // NSA_HEAD_DIM
NSA_KV_HEADS = 4
NSA_GROUP = NSA_Q_HEADS // NSA_KV_HEADS
CMP_BLOCK = 32
CMP_STRIDE = 16
CMP_HIDDEN = 256
SEL_BLOCK = 64
N_SELECT = 16
N_LOCAL = 2
WINDOW = 512
Q_BLOCK = 128
SEL_Q_BLOCK = 32
FORCE_SCORE = 1e9
NEG_INF = -1e30
PEER_HEADS = 8
N_KEYS = 128
N_EXPERTS = N_KEYS * N_KEYS
PEER_KEY_DIM = 256
PEER_TOPK = 16
PEER_TOKEN_BLOCK = 128
NORM_EPS = 1e-6

KV_W = NSA_KV_HEADS * NSA_HEAD_DIM
N_GATES = 3 * NSA_Q_HEADS
IN_SIZES = [HG_WIDTH, HG_WIDTH, HG_WIDTH, HG_WIDTH,
            NSA_WIDTH,
            KV_W, KV_W, KV_W, KV_W, KV_W, KV_W,
            N_GATES]
IN_COLS = sum(IN_SIZES)

kernel_name = "hymba_hgrn2_nsa_peer"


def rms_norm(x, w):
    xf = x.astype(jnp.float32)
    y = xf * lax.rsqrt(jnp.mean(xf * xf, axis=-1, keepdims=True) + NORM_EPS)
    return (y * w.astype(jnp.float32)).astype(x.dtype)


def masked_softmax(s, mask):
    p = jax.nn.softmax(jnp.where(mask, s, NEG_INF), axis=-1)
    return jnp.where(mask, p, 0.0)


def alibi_slopes(n):
    return jnp.asarray(2.0 ** (-8.0 * np.arange(1, n + 1) / n), dtype=jnp.float32)


def hgrn2_mixer(q, f_logit, i_in, g, lb, norm_w):
    B, T, _ = q.shape
    H, dk, C = HG_HEADS, HG_HEAD_DIM, HG_CHUNK
    nc = T // C
    f = lb + (1.0 - lb) * jax.nn.sigmoid(f_logit.astype(jnp.float32))

    def chunks(a):
        return a.astype(jnp.float32).reshape(B, nc, C, H, dk).transpose(0, 3, 1, 2, 4)

    qc, kc, vc, lfc = chunks(q), chunks(1.0 - f), chunks(i_in), chunks(jnp.log(f))
    b = jnp.cumsum(lfc, axis=3)
    b_end = b[:, :, :, -1:, :]
    q_dec = qc * jnp.exp(b)
    causal = np.tril(np.ones((C, C), dtype=bool))
    attn = jnp.where(causal, jnp.einsum('bhntc,bhnsc->bhnts', q_dec, kc * jnp.exp(-b)), 0.0)
    o_intra = jnp.einsum('bhnts,bhnsv->bhntv', attn, vc)
    upd = jnp.einsum('bhnsc,bhnsv->bhncv', kc * jnp.exp(b_end - b), vc)
    dec = jnp.exp(b_end[:, :, :, 0, :])

    def step(state, inp):
        d, u = inp
        return d[..., None] * state + u, state

    s0 = jnp.zeros((B, H, dk, dk), jnp.float32)
    _, s_prev = lax.scan(step, s0, (jnp.moveaxis(dec, 2, 0), jnp.moveaxis(upd, 2, 0)))
    o_inter = jnp.einsum('bhntc,nbhcv->bhntv', q_dec, s_prev)
    o = (o_intra + o_inter).transpose(0, 2, 3, 1, 4).reshape(B, T, H, dk)
    o = rms_norm(o, norm_w) * jax.nn.silu(g.astype(jnp.float32).reshape(B, T, H, dk))
    return o.reshape(B, T, H * dk).astype(q.dtype)


def nsa_mixer(q, kc, vc, ks, vs, kw, vw, gate_logits, q_norm_w, kc_norm_w, ks_norm_w, kw_norm_w,
              pos_k, pos_v, w_ck1, w_ck2, w_cv1, w_cv2):
    B, T, _ = q.shape
    Hkv, G, dh = NSA_KV_HEADS, NSA_GROUP, NSA_HEAD_DIM
    f32 = jnp.float32
    slopes = alibi_slopes(NSA_Q_HEADS).reshape(Hkv, G)
    qn = rms_norm(q.reshape(B, T, Hkv, G, dh), q_norm_w).transpose(0, 2, 3, 1, 4) * (dh ** -0.5)

    def kv_heads(a):
        return a.reshape(B, T, Hkv, dh).transpose(0, 2, 1, 3)

    t_pos = np.arange(T)

    n_cmp = (T - CMP_BLOCK) // CMP_STRIDE + 1
    cmp_start = np.arange(n_cmp) * CMP_STRIDE
    blk_idx = cmp_start[:, None] + np.arange(CMP_BLOCK)[None, :]
    cmp_end = cmp_start + CMP_BLOCK - 1

    def compress(a, pos, w1, w2):
        blocks = kv_heads(a)[:, :, blk_idx] + pos
        flat = blocks.reshape(B, Hkv, n_cmp, CMP_BLOCK * dh)
        return jax.nn.gelu(flat @ w1) @ w2

    k_cmp = rms_norm(compress(kc, pos_k, w_ck1, w_ck2), kc_norm_w)
    v_cmp = compress(vc, pos_v, w_cv1, w_cv2)
    dist_c = t_pos[:, None] - cmp_end[None, :]
    s_c = jnp.einsum('bgrtd,bgnd->bgrtn', qn, k_cmp).astype(f32) \
        - slopes[:, :, None, None] * dist_c.astype(np.float32)
    p_cmp = masked_softmax(s_c, dist_c >= 0)
    o_cmp = jnp.einsum('bgrtn,bgnd->bgrtd', p_cmp, v_cmp)

    n_sel = T // SEL_BLOCK
    sel_start = np.arange(n_sel) * SEL_BLOCK
    overlap = ((cmp_start[:, None] < sel_start[None, :] + SEL_BLOCK)
               & (cmp_start[:, None] + CMP_BLOCK > sel_start[None, :])).astype(np.float32)
    imp = jnp.einsum('bgrtn,nj->bgtj', p_cmp, overlap)
    cur = t_pos // SEL_BLOCK
    j = np.arange(n_sel)
    forced = (j[None, :] == 0) | ((j[None, :] <= cur[:, None]) & (j[None, :] > cur[:, None] - N_LOCAL))
    future = j[None, :] > cur[:, None]
    imp = jnp.where(forced, FORCE_SCORE, jnp.where(future, -FORCE_SCORE, imp))
    n_top = min(N_SELECT, n_sel)
    _, sel_idx = lax.top_k(imp, n_top)

    k_sel = kv_heads(rms_norm(ks.reshape(B, T, Hkv * 1, dh), ks_norm_w).reshape(B, T, KV_W)) \
        .reshape(B, Hkv, n_sel, SEL_BLOCK, dh)
    v_sel = kv_heads(vs).reshape(B, Hkv, n_sel, SEL_BLOCK, dh)
    bi = jnp.arange(B)[:, None, None, None]
    gi = jnp.arange(Hkv)[None, :, None, None]
    m_keys = n_top * SEL_BLOCK

    def sel_block(args):
        q_b, idx_b, start = args
        k_g = k_sel[bi, gi, idx_b].reshape(B, Hkv, SEL_Q_BLOCK, m_keys, dh)
        v_g = v_sel[bi, gi, idx_b].reshape(B, Hkv, SEL_Q_BLOCK, m_keys, dh)
        key_pos = (idx_b[..., None] * SEL_BLOCK + jnp.arange(SEL_BLOCK)).reshape(B, Hkv, SEL_Q_BLOCK, m_keys)
        dist = ((start + jnp.arange(SEL_Q_BLOCK))[:, None] - key_pos)[:, :, None]
        s = jnp.einsum('bgrqd,bgqmd->bgrqm', q_b, k_g).astype(f32) \
            - slopes[None, :, :, None, None] * dist.astype(f32)
        p = masked_softmax(s, dist >= 0)
        return jnp.einsum('bgrqm,bgqmd->bgrqd', p, v_g)

    n_qb = T // SEL_Q_BLOCK
    q_blocks = qn.reshape(B, Hkv, G, n_qb, SEL_Q_BLOCK, dh).transpose(3, 0, 1, 2, 4, 5)
    idx_blocks = sel_idx.reshape(B, Hkv, n_qb, SEL_Q_BLOCK, n_top).transpose(2, 0, 1, 3, 4)
    starts = jnp.arange(n_qb, dtype=jnp.int32) * SEL_Q_BLOCK
    o_sel = lax.map(sel_block, (q_blocks, idx_blocks, starts))
    o_sel = o_sel.transpose(1, 2, 3, 0, 4, 5).reshape(B, Hkv, G, T, dh)

    pad = ((0, 0), (0, 0), (WINDOW, 0), (0, 0))
    k_win = jnp.pad(kv_heads(rms_norm(kw.reshape(B, T, Hkv, dh), kw_norm_w).reshape(B, T, KV_W)), pad)
    v_win = jnp.pad(kv_heads(vw), pad)
    span = Q_BLOCK + WINDOW
    dist_w = np.arange(Q_BLOCK)[:, None] + WINDOW - np.arange(span)[None, :]

    def win_block(args):
        q_b, start = args
        k_b = lax.dynamic_slice_in_dim(k_win, start, span, axis=2)
        v_b = lax.dynamic_slice_in_dim(v_win, start, span, axis=2)
        key_pos = start - WINDOW + jnp.arange(span)
        mask = (dist_w >= 0) & (dist_w < WINDOW) & (key_pos >= 0)[None, :]
        s = jnp.einsum('bgrqd,bgkd->bgrqk', q_b, k_b).astype(f32) \
            - slopes[:, :, None, None] * dist_w.astype(np.float32)
        p = masked_softmax(s, mask)
        return jnp.einsum('bgrqk,bgkd->bgrqd', p, v_b)

    n_wb = T // Q_BLOCK
    q_wblocks = qn.reshape(B, Hkv, G, n_wb, Q_BLOCK, dh).transpose(3, 0, 1, 2, 4, 5)
    w_starts = jnp.arange(n_wb, dtype=jnp.int32) * Q_BLOCK
    o_win = lax.map(win_block, (q_wblocks, w_starts))
    o_win = o_win.transpose(1, 2, 3, 0, 4, 5).reshape(B, Hkv, G, T, dh)

    gates = jax.nn.sigmoid(gate_logits.astype(f32)).reshape(B, T, Hkv, G, 3).transpose(0, 2, 3, 1, 4)
    o = gates[..., 0:1] * o_cmp + gates[..., 1:2] * o_sel + gates[..., 2:3] * o_win
    return o.transpose(0, 3, 1, 2, 4).reshape(B, T, NSA_WIDTH).astype(q.dtype)


def peer_ffn(x, w_q, sub_keys, u_tab, v_tab):
    B, T, D = x.shape
    n = B * T
    H, K = PEER_HEADS, PEER_TOPK
    xt = x.reshape(n, D)
    q = (xt @ w_q).reshape(n, H, 2, PEER_KEY_DIM // 2)
    s = jnp.einsum('nhpd,hpkd->nhpk', q, sub_keys).astype(jnp.float32)
    s_top, i_top = lax.top_k(s, K)
    cand_s = (s_top[:, :, 0, :, None] + s_top[:, :, 1, None, :]).reshape(n, H, K * K)
    cand_i = (i_top[:, :, 0, :, None] * N_KEYS + i_top[:, :, 1, None, :]).reshape(n, H, K * K)
    best_s, best_pos = lax.top_k(cand_s, K)
    expert = jnp.take_along_axis(cand_i, best_pos, axis=-1)
    gate = jax.nn.softmax(best_s, axis=-1).astype(x.dtype)
    nb = n // PEER_TOKEN_BLOCK

    def block(args):
        xb, eb, gb = args
        act = jax.nn.gelu(jnp.einsum('td,thkd->thk', xb, u_tab[eb]))
        return jnp.einsum('thk,thkd->td', gb * act, v_tab[eb])

    out = lax.map(block, (xt.reshape(nb, PEER_TOKEN_BLOCK, D),
                          expert.reshape(nb, PEER_TOKEN_BLOCK, H, K),
                          gate.reshape(nb, PEER_TOKEN_BLOCK, H, K)))
    return out.reshape(B, T, D)


def setup_inputs(seed: int = 0) -> dict:
    key = jax.random.key(seed)
    ks = jax.random.split(key, 24)
    f32 = jnp.float32
    L, dh = DEPTH, NSA_HEAD_DIM

    def nrm(k, shape, scale):
        return jax.random.normal(k, shape, f32) * scale

    def gain(k, shape):
        return 1.0 + 0.01 * jax.random.normal(k, shape, f32)

    return {
        "x": nrm(ks[0], (BATCH, SEQ, D_MODEL), 1.0),
        "norm1_w": gain(ks[1], (L, D_MODEL)),
        "w_in": nrm(ks[2], (L, D_MODEL, IN_COLS), D_MODEL ** -0.5),
        "hg_lb_logits": nrm(ks[3], (L + 1, HG_WIDTH), 0.1),
        "hg_norm_w": gain(ks[4], (L, HG_HEAD_DIM)),
        "q_norm_w": gain(ks[5], (L, dh)),
        "kc_norm_w": gain(ks[6], (L, dh)),
        "ks_norm_w": gain(ks[7], (L, dh)),
        "kw_norm_w": gain(ks[8], (L, dh)),
        "cmp_pos_k": nrm(ks[9], (L, CMP_BLOCK, dh), 0.1),
        "cmp_pos_v": nrm(ks[10], (L, CMP_BLOCK, dh), 0.1),
        "w_ck1": nrm(ks[11], (L, CMP_BLOCK * dh, CMP_HIDDEN), (CMP_BLOCK * dh) ** -0.5),
        "w_ck2": nrm(ks[12], (L, CMP_HIDDEN, dh), CMP_HIDDEN ** -0.5),
        "w_cv1": nrm(ks[13], (L, CMP_BLOCK * dh, CMP_HIDDEN), (CMP_BLOCK * dh) ** -0.5),
        "w_cv2": nrm(ks[14], (L, CMP_HIDDEN, dh), CMP_HIDDEN ** -0.5),
        "w_out": nrm(ks[15], (L, MIX_WIDTH, D_MODEL), MIX_WIDTH ** -0.5),
        "norm2_w": gain(ks[16], (L, D_MODEL)),
        "peer_w_q": nrm(ks[17], (L, D_MODEL, PEER_HEADS * PEER_KEY_DIM), D_MODEL ** -0.5),
        "peer_sub_keys": nrm(ks[18], (L, PEER_HEADS, 2, N_KEYS, PEER_KEY_DIM // 2), (PEER_KEY_DIM // 2) ** -0.5),
        "peer_u": nrm(ks[19], (L, N_EXPERTS, D_MODEL), D_MODEL ** -0.5),
        "peer_v": nrm(ks[20], (L, N_EXPERTS, D_MODEL), PEER_HEADS ** -0.5),
    }


def reference(x, norm1_w, w_in, hg_lb_logits, hg_norm_w, q_norm_w, kc_norm_w, ks_norm_w, kw_norm_w,
              cmp_pos_k, cmp_pos_v, w_ck1, w_ck2, w_cv1, w_cv2, w_out, norm2_w,
              peer_w_q, peer_sub_keys, peer_u, peer_v):
    split_at = np.cumsum(IN_SIZES)[:-1].tolist()
    lower_bounds = jnp.cumsum(jax.nn.softmax(hg_lb_logits.astype(jnp.float32), axis=0), axis=0)
    h = x
    for layer in range(DEPTH):
        hn = rms_norm(h, norm1_w[layer])
        (hq, hf, hi, hg, nq, nkc, nvc, nks, nvs, nkw, nvw, ngate) = jnp.split(hn @ w_in[layer], split_at, axis=-1)
        hg_out = hgrn2_mixer(hq, hf, hi, hg, lower_bounds[layer], hg_norm_w[layer])
        nsa_out = nsa_mixer(nq, nkc, nvc, nks, nvs, nkw, nvw, ngate,
                            q_norm_w[layer], kc_norm_w[layer], ks_norm_w[layer], kw_norm_w[layer],
                            cmp_pos_k[layer], cmp_pos_v[layer], w_ck1[layer], w_ck2[layer],
                            w_cv1[layer], w_cv2[layer])
        h = h + jnp.concatenate([hg_out, nsa_out], axis=-1) @ w_out[layer]
        h = h + peer_ffn(rms_norm(h, norm2_w[layer]), peer_w_q[layer], peer_sub_keys[layer],
                         peer_u[layer], peer_v[layer])
    return h
```

```python
import contextlib
import numpy as np
import ml_dtypes
import concourse.bass as bass
import concourse.mybir as mybir
from concourse.bass_utils import run_bass_kernel_spmd

F32 = mybir.dt.float32
BF16 = mybir.dt.bfloat16
U32 = mybir.dt.uint32
AF = mybir.ActivationFunctionType
ALU = mybir.AluOpType
AX = mybir.AxisListType

T = 2048
D = 2048
NT = 16
EPS = 1e-6
NEG = -30000.0
C_HQ, C_HF, C_HI, C_HG, C_NQ, C_KC, C_VC, C_KS, C_VS, C_KW, C_VW, C_GT = (
    0, 1024, 2048, 3072, 4096, 5120, 5376, 5632, 5888, 6144, 6400, 6656)
IN_COLS = 6704


class Res:
    def __init__(self, name):
        self.name = name
        self.w = None
        self.r = {}
        self.dsem = None
        self.dcnt = 0


class Eng:
    def __init__(self, name, h, sem):
        self.name = name
        self.h = h
        self.sem = sem
        self.cnt = 0
        self.seen = {}


class KB:
    def __init__(self, nc, stack):
        self.nc = nc
        self.stack = stack
        self.nsem = 0
        self.all_res = []
        self.E = {}
        for name, h in (("pe", nc.tensor), ("dve", nc.vector), ("act", nc.scalar),
                        ("pool", nc.gpsimd), ("sp", nc.sync)):
            self.E[name] = Eng(name, h, self.newsem("e_" + name))

    def newsem(self, name):
        self.nsem += 1
        return self.stack.enter_context(self.nc.semaphore(name))

    def res(self, name):
        r = Res(name)
        self.all_res.append(r)
        return r

    def barrier(self):
        evs = []
        for F in self.E.values():
            if F.cnt > 0:
                evs.append((F.name, F.sem, F.cnt))
        for r in self.all_res:
            if r.dsem is not None and r.dcnt > 0:
                evs.append(("d_" + r.name, r.dsem, r.dcnt))
        for E in self.E.values():
            for ev in evs:
                if ev[0] != E.name:
                    self._wait(E, ev)

    def _wait(self, E, ev):
        key, sem, val = ev
        if E.seen.get(key, 0) < val:
            E.h.wait_ge(sem, val)
            E.seen[key] = val

    def _deps(self, E, reads, writes):
        for r in reads:
            if r.w is not None:
                if not (r.w[0] == E.name and E.name == "pe"):
                    self._wait(E, r.w)
        for w in writes:
            if w.w is not None:
                if not (w.w[0] == E.name and E.name == "pe"):
                    self._wait(E, w.w)
            for ev in w.r.values():
                if ev[0] != E.name:
                    self._wait(E, ev)

    def _mark(self, ev, reads, writes):
        for r in reads:
            r.r[ev[0]] = ev
        for w in writes:
            w.w = ev
            w.r = {}

    def op(self, eng, fn, reads=(), writes=()):
        E = self.E[eng]
        self._deps(E, reads, writes)
        ins = fn(E.h)
        E.cnt += 1
        ins.then_inc(E.sem, 1)
        self._mark((E.name, E.sem, E.cnt), reads, writes)

    def dma(self, eng, q, fn, reads=(), writes=()):
        E = self.E[eng]
        self._deps(E, reads, writes)
        if q.dsem is None:
            q.dsem = self.newsem("d_" + q.name)
        ins = fn(E.h)
        q.dcnt += 16
        ins.then_inc(q.dsem, 16)
        self._mark(("d_" + q.name, q.dsem, q.dcnt), reads, writes)

    def wait_all(self, eng, ress):
        E = self.E[eng]
        for r in ress:
            if r.w is not None:
                self._wait(E, r.w)
            for ev in r.r.values():
                self._wait(E, ev)


def _bf16_split3(v):
    v = np.asarray(v, np.float64)
    hi = v.astype(ml_dtypes.bfloat16).astype(np.float64)
    lo = (v - hi).astype(ml_dtypes.bfloat16).astype(np.float64)
    lo2 = (v - hi - lo).astype(ml_dtypes.bfloat16).astype(np.float64)
    return hi, lo, lo2


def host_tables():
    tb = {}
    tb["ident"] = np.eye(128, dtype=np.float32)
    s = np.arange(128)
    tb["triLE"] = (s[:, None] <= s[None, :]).astype(np.float32)
    tb["triGT"] = (s[:, None] > s[None, :]).astype(np.float32)
    blk = np.zeros((128, 128), np.float32)
    blk[:64, :64] = 1
    blk[64:, 64:] = 1
    tb["onesblk"] = blk
    slopes = 2.0 ** (-8.0 * np.arange(1, 17) / 16)
    slopes = slopes.astype(np.float32).astype(np.float64)
    hi, lo, lo2 = _bf16_split3(slopes)
    sl = np.zeros((9, 16, 128), np.float32)
    for k in range(3):
        sl[3 * k + 0] = hi[:, None]
        sl[3 * k + 1] = lo[:, None]
        sl[3 * k + 2] = lo2[:, None]
    tb["slopeR"] = sl.reshape(9, 16 * 128)
    pl = np.zeros((9, 16, 128), np.float32)
    for dl in range(16):
        pl[0:3, dl, :] = -128.0 * (dl + 1)
        pl[3:6, dl, :] = s[None, :] + 64.0
    tb["posL"] = pl.reshape(9, 16 * 128)
    pc = np.zeros((9, 16, 128), np.float32)
    for tt in range(16):
        pc[0:3, tt, :] = -128.0 * tt
        pc[3:6, tt, :] = 16.0 * s[None, :]
        pc[6:9, tt, :] = -25.0
    tb["posC"] = pc.reshape(9, 16 * 128)
    n = np.arange(128)
    mc = np.zeros((128, 16, 128), np.float32)
    for tt in range(16):
        mc[:, tt, :] = (16 * n[:, None] + 31 <= 128 * tt + s[None, :])
    mc[127] = 0
    tb["maskC"] = mc.reshape(128, 16 * 128)
    t_pos = np.arange(T)
    cur = t_pos // 64
    j = np.arange(32)
    forced = (j[None, :] == 0) | ((j[None, :] <= cur[:, None]) & (j[None, :] > cur[:, None] - 2))
    future = j[None, :] > cur[:, None]
    m1 = np.where(forced | future, 0.0, 1.0).astype(np.float32)
    m2 = np.where(forced, 1e9, np.where(future, -1e9, 0.0)).astype(np.float32)
    tb["M1"] = m1.reshape(16, 128, 32).transpose(1, 0, 2).reshape(128, 512).copy()
    tb["M2"] = m2.reshape(16, 128, 32).transpose(1, 0, 2).reshape(128, 512).copy()
    E = np.zeros((32, 16, 128), np.float32)
    for st in range(16):
        E[2 * st, st, :64] = 1
        E[2 * st + 1, st, 64:] = 1
    tb["Esel"] = E.reshape(32, 16 * 128)
    cmp_start = np.arange(127) * 16
    sel_start = np.arange(32) * 64
    ov = ((cmp_start[:, None] < sel_start[None, :] + 64)
          & (cmp_start[:, None] + 32 > sel_start[None, :])).astype(np.float32)
    ovp = np.zeros((128, 32), np.float32)
    ovp[:127] = ov
    tb["ovl"] = ovp
    tb["iota16"] = np.tile(np.arange(16, dtype=np.float32)[None, :], (128, 1))
    return tb


TABLE_SHAPES = {k: v.shape for k, v in host_tables().items()}


def build(debug=False, NH=8, NG=4, ND=16, NE=16, dumps=False):
    nc = bass.Bass("TRN2", target_bir_lowering=False)
    di = {}
    dump_list = []

    def din(name, shape, dt=F32):
        di[name] = nc.dram_tensor(name, list(shape), dt, kind="ExternalInput").ap()
        return di[name]

    x = din("x", [T, D])
    norm1_w = din("norm1_w", [1, D])
    w_in = din("w_in", [1, D, IN_COLS])
    hg_lb = din("hg_lb_logits", [2, 1024])
    hg_norm_w = din("hg_norm_w", [1, 128])
    q_norm_w = din("q_norm_w", [1, 64])
    kc_norm_w = din("kc_norm_w", [1, 64])
    ks_norm_w = din("ks_norm_w", [1, 64])
    kw_norm_w = din("kw_norm_w", [1, 64])
    cmp_pos_k = din("cmp_pos_k", [1, 32, 64])
    cmp_pos_v = din("cmp_pos_v", [1, 32, 64])
    w_ck1 = din("w_ck1", [1, 2048, 256])
    w_ck2 = din("w_ck2", [1, 256, 64])
    w_cv1 = din("w_cv1", [1, 2048, 256])
    w_cv2 = din("w_cv2", [1, 256, 64])
    w_out = din("w_out", [1, 2048, 2048])
    norm2_w = din("norm2_w", [1, D])
    peer_w_q = din("peer_w_q", [1, D, 2048])
    peer_sub_keys = din("peer_sub_keys", [1, 8, 2, 128, 128])
    peer_u = din("peer_u", [1, 16384 if NE else 8, D])
    peer_v = din("peer_v", [1, 16384 if NE else 8, D])
    tabs = {k: din("tb_" + k, shp) for k, shp in TABLE_SHAPES.items()}
    y = nc.dram_tensor("y", [T, D], F32, kind="ExternalOutput").ap()
    omix = nc.dram_tensor("omix", [2048, T], BF16, kind="Internal").ap()
    hscr = nc.dram_tensor("hscr", [T, D], F32, kind="Internal").ap()
    h2scr = nc.dram_tensor("h2scr", [T, D], F32, kind="Internal").ap()
    NEXP = 16384 if NE else 8
    uvbf = nc.dram_tensor("uvbf", [NEXP, 2 * D], BF16, kind="Internal").ap()
    dbg = {}
    if debug:
        dbg["omix_o"] = nc.dram_tensor("omix_o", [2048, T], BF16, kind="ExternalOutput").ap()
        dbg["h_o"] = nc.dram_tensor("h_o", [T, D], F32, kind="ExternalOutput").ap()
        dbg["eidx_o"] = nc.dram_tensor("eidx_o", [128, 16 * 128], U32, kind="ExternalOutput").ap()
        dbg["gate_o"] = nc.dram_tensor("gate_o", [128, 16 * 128], F32, kind="ExternalOutput").ap()

    with contextlib.ExitStack() as top:
        kb = KB(nc, top)
        R = kb.res

        def sb(stack, name, shape, dt):
            t = stack.enter_context(nc.sbuf_tensor(name, list(shape), dt))
            return t

        PS = []
        PR = []
        for i in range(8):
            PS.append(top.enter_context(nc.psum_tensor("ps%d" % i, [128, 512], F32)))
            PR.append(R("ps%d" % i))

        ident_f = sb(top, "ident_f", [128, 128], F32)
        ident_b = sb(top, "ident_b", [128, 128], BF16)
        ones_b = sb(top, "ones_b", [128, 128], BF16)
        onesblk_b = sb(top, "onesblk_b", [128, 128], BF16)
        triLE_b = sb(top, "triLE_b", [128, 128], BF16)
        triGT_b = sb(top, "triGT_b", [128, 128], BF16)
        zrow = sb(top, "zrow", [1, 512], BF16)
        r_const = R("const")
        r_constp = R("constp")
        r_omix = R("omix")
        r_hscr = R("hscr")
        r_h2scr = R("h2scr")
        r_y = R("y")
        r_ubf = R("ubf")
        r_vbf = R("vbf")
        conv_jobs = []
        if NE:
            for i in range(32):
                conv_jobs.append((r_ubf, uvbf[:, 0:D], peer_u, i))
                conv_jobs.append((r_ubf, uvbf[:, D:2 * D], peer_v, i))

        def issue_conv(n):
            for _ in range(n):
                if not conv_jobs:
                    return
                rr, dst, src, i = conv_jobs.pop(0)
                kb.dma("pool", rr, lambda e: e.dma_start(out=dst[i * 512:(i + 1) * 512, :],
                                                         in_=src[0, i * 512:(i + 1) * 512, :]), writes=[rr])

        def cload(dst, src, cast):
            if cast:
                kb.dma("pool", r_constp, lambda e: e.dma_start(out=dst, in_=src), writes=[r_constp])
            else:
                kb.dma("sp", r_const, lambda e: e.dma_start(out=dst, in_=src), writes=[r_const])

        def dump(name, ap, shape, dt, reads):
            if not dumps:
                return
            o = nc.dram_tensor("dump_" + name, list(shape), dt, kind="ExternalOutput").ap()
            dump_list.append(name)
            kb.dma("sp", r_const, lambda e: e.dma_start(out=o, in_=ap), reads=list(reads), writes=[r_y])

        cload(ident_f[:], tabs["ident"][:, :], False)
        cload(ident_b[:], tabs["ident"][:, :], True)
        cload(onesblk_b[:], tabs["onesblk"][:, :], True)
        cload(triLE_b[:], tabs["triLE"][:, :], True)
        cload(triGT_b[:], tabs["triGT"][:, :], True)
        kb.op("dve", lambda e: e.memset(ones_b[:], 1.0), writes=[r_const])
        kb.op("dve", lambda e: e.memset(zrow[:], 0.0), writes=[r_const])
        kb.barrier()

        eidx = sb(top, "eidx", [128, 16, 128], U32)
        gate = sb(top, "gate", [128, 16, 128], F32)
        r_eidx = [R("eidx%d" % i) for i in range(16)]
        r_gate = [R("gate%d" % i) for i in range(16)]

        with contextlib.ExitStack() as ms:
            xnT = sb(ms, "xnT", [128, 16, T], BF16)
            r_xnT = R("xnT")
            wslot = [sb(ms, "wslot%d" % i, [128, 16, 512], BF16) for i in range(2)]
            r_wslot = [R("wslot%d" % i) for i in range(2)]
            wcnt = [0]

            def load_wcols(pieces):
                si = wcnt[0] % 2
                wcnt[0] += 1
                off = 0
                for (c0, n) in pieces:
                    src = w_in[0, :, c0:c0 + n].rearrange("(c p) n -> p c n", p=128)
                    dst = wslot[si][:, :, off:off + n]
                    kb.dma("pool", r_wslot[si], lambda e, d=dst, s_=src: e.dma_start(out=d, in_=s_),
                           writes=[r_wslot[si]])
                    off += n
                return wslot[si], r_wslot[si]

            with contextlib.ExitStack() as pa:
                w1bc = sb(pa, "w1bc", [128, D], F32)
                r_w1bc = R("w1bc")
                kb.dma("sp", r_w1bc, lambda e: e.dma_start(out=w1bc[:], in_=norm1_w[0].partition_broadcast(128)),
                       writes=[r_w1bc])
                xt = [sb(pa, "xt%d" % i, [128, D], F32) for i in range(2)]
                r_xt = [R("xt%d" % i) for i in range(2)]
                xnb = [sb(pa, "xnb%d" % i, [128, D], BF16) for i in range(2)]
                r_xnb = [R("xnb%d" % i) for i in range(2)]
                junk = sb(pa, "junkA", [128, D], F32)
                r_junk = R("junkA")
                st = sb(pa, "statA", [128, 16, 4], F32)
                r_st = [R("statA%d" % i) for i in range(16)]
                for tt in range(NT):
                    s_ = tt % 2
                    kb.dma("sp", r_xt[s_], lambda e: e.dma_start(out=xt[s_][:], in_=x[tt * 128:(tt + 1) * 128, :]),
                           writes=[r_xt[s_]])
                    kb.op("act", lambda e: e.activation(out=junk[:], in_=xt[s_][:], func=AF.Square),
                          reads=[r_xt[s_]], writes=[r_junk])
                    kb.op("dve", lambda e: e.tensor_reduce(out=st[:, tt, 0:1], in_=junk[:], axis=AX.X, op=ALU.add),
                          reads=[r_junk], writes=[r_st[tt]])
                    kb.op("act", lambda e: e.activation(out=st[:, tt, 1:2], in_=st[:, tt, 0:1], func=AF.Sqrt,
                                                        scale=1.0 / D, bias=EPS),
                          reads=[r_st[tt]], writes=[r_st[tt]])
                    kb.op("dve", lambda e: e.reciprocal(out=st[:, tt, 2:3], in_=st[:, tt, 1:2]),
                          reads=[r_st[tt]], writes=[r_st[tt]])
                    kb.op("dve", lambda e: e.scalar_tensor_tensor(out=xnb[s_][:], in0=xt[s_][:], scalar=st[:, tt, 2:3],
                                                                  in1=w1bc[:], op0=ALU.mult, op1=ALU.mult),
                          reads=[r_xt[s_], r_st[tt], r_w1bc], writes=[r_xnb[s_]])
                    for half in range(2):
                        bk = half
                        pv = PS[bk][:].bitcast(BF16)
                        for j in range(8):
                            c = half * 8 + j
                            kb.op("pe", lambda e: e.transpose(out=pv[:, j * 128:(j + 1) * 128],
                                                              in_=xnb[s_][:, c * 128:(c + 1) * 128],
                                                              identity=ident_b[:]),
                                  reads=[r_xnb[s_], r_const], writes=[PR[bk]])
                        eng = "act" if half == 0 else "dve"
                        dst = xnT[:, half * 8:(half + 1) * 8, tt * 128:(tt + 1) * 128]
                        src = pv.rearrange("p (j t) -> p j t", j=8)
                        if eng == "act":
                            kb.op("act", lambda e: e.copy(out=dst, in_=src), reads=[PR[bk]], writes=[r_xnT])
                        else:
                            kb.op("dve", lambda e: e.tensor_copy(out=dst, in_=src), reads=[PR[bk]], writes=[r_xnT])

                dump("w1bc", w1bc[:], [128, D], F32, [r_w1bc])
                dump("xt1", xt[1][:], [128, D], F32, [r_xt[1]])
                dump("xnb1", xnb[1][:], [128, D], BF16, [r_xnb[1]])
                dump("identb", ident_b[:], [128, 128], BF16, [r_const])
                dump("stA", st[:].rearrange("p a b -> p (a b)"), [128, 64], F32, r_st)
            kb.barrier()
            with contextlib.ExitStack() as pb:
                lbt = sb(pb, "lbt", [128, 8, 4], F32)
                r_lbt = R("lbt")
                for two in range(2):
                    kb.dma("sp", r_lbt, lambda e: e.dma_start(
                        out=lbt[:, :, two],
                        in_=hg_lb[two].rearrange("(h p) -> p h", p=128),
                        allow_slow_non_contiguous=True), writes=[r_lbt])
                kb.op("dve", lambda e: e.tensor_tensor(out=lbt[:, :, 2:3], in0=lbt[:, :, 0:1], in1=lbt[:, :, 1:2],
                                                       op=ALU.subtract), reads=[r_lbt], writes=[r_lbt])
                kb.op("act", lambda e: e.activation(out=lbt[:, :, 2:3], in_=lbt[:, :, 2:3], func=AF.Sigmoid),
                      reads=[r_lbt], writes=[r_lbt])
                kb.op("dve", lambda e: e.tensor_scalar(out=lbt[:, :, 3:4], in0=lbt[:, :, 2:3], scalar1=-1.0,
                                                       scalar2=1.0, op0=ALU.mult, op1=ALU.add),
                      reads=[r_lbt], writes=[r_lbt])
                hgw = sb(pb, "hgw", [128, 1], F32)
                kb.dma("sp", r_lbt, lambda e: e.dma_start(out=hgw[:], in_=hg_norm_w.rearrange("o p -> p o"),
                                                           allow_slow_non_contiguous=True), writes=[r_lbt])
                rmask = sb(pb, "rmask", [128, T], F32)
                r_rmask = R("rmask")
                kb.op("pool", lambda e: e.memset(rmask[:], 1.0), writes=[r_rmask])
                kb.op("pool", lambda e: e.memset(rmask[:].rearrange("p (n c) -> p n c", c=128)[:, :, 0:1], 0.0),
                      writes=[r_rmask])
                bufA = sb(pb, "hA", [128, T], F32)
                bufB = sb(pb, "hB", [128, T], F32)
                bufC = sb(pb, "hC", [128, T], F32)
                bufD = sb(pb, "hD", [128, T], F32)
                rA, rB, rC, rD = R("hA"), R("hB"), R("hC"), R("hD")
                qd = sb(pb, "qd", [128, T], BF16)
                kd = sb(pb, "kd", [128, T], BF16)
                sg = sb(pb, "sg", [128, T], BF16)
                r_qd, r_kd, r_sg = R("qd"), R("kd"), R("sg")
                vtok = sb(pb, "vtok", [128, 16, 128], BF16)
                kdtok = sb(pb, "kdtok", [128, 16, 128], BF16)
                r_vtok, r_kdtok = R("vtok"), R("kdtok")
                dec = sb(pb, "dec", [128, 16], F32)
                r_dec = R("dec")
                S32 = sb(pb, "S32", [128, 128], F32)
                Sbf = sb(pb, "Sbf", [128, 128], BF16)
                Stmp = sb(pb, "Stmp", [128, 128], F32)
                r_S32, r_Sbf, r_Stmp = R("S32"), R("Sbf"), R("Stmp")
                at = [sb(pb, "at%d" % i, [128, 128], BF16) for i in range(2)]
                r_at = [R("at%d" % i) for i in range(2)]
                sq = [sb(pb, "sq%d" % i, [128, 128], BF16) for i in range(2)]
                r_sq = [R("sq%d" % i) for i in range(2)]
                lnv = [sb(pb, "lnv%d" % i, [128, 128], F32) for i in range(2)]
                r_lnv = [R("lnv%d" % i) for i in range(2)]
                t1 = [sb(pb, "t1%d" % i, [128, 128], F32) for i in range(2)]
                r_t1 = [R("t1%d" % i) for i in range(2)]
                ost = [sb(pb, "ost%d" % i, [128, T], BF16) for i in range(2)]
                r_ost = [R("ost%d" % i) for i in range(2)]

                wl_next = load_wcols([(C_HQ, 128), (C_HF, 128), (C_HI, 128), (C_HG, 128)])
                for h in range(NH):
                    issue_conv(8)
                    wl, r_wl = wl_next
                    if h < NH - 1:
                        o_ = (h + 1) * 128
                        wl_next = load_wcols([(C_HQ + o_, 128), (C_HF + o_, 128), (C_HI + o_, 128), (C_HG + o_, 128)])
                    bkc = 0
                    for qi, kind in ((0, "q"), (1, "f"), (3, "g")):
                        for tb_ in range(4):
                            bk = bkc % 4
                            bkc += 1
                            for c in range(16):
                                kb.op("pe", lambda e: e.matmul(PS[bk][:], lhsT=wl[:, c, qi * 128:(qi + 1) * 128],
                                                               rhs=xnT[:, c, tb_ * 512:(tb_ + 1) * 512],
                                                               start=(c == 0), stop=(c == 15)),
                                      reads=[r_wl, r_xnT], writes=[PR[bk]])
                            sl_ = slice(tb_ * 512, (tb_ + 1) * 512)
                            if kind == "q":
                                kb.op("dve", lambda e: e.tensor_copy(out=bufA[:, sl_], in_=PS[bk][:]),
                                      reads=[PR[bk]], writes=[rA])
                            elif kind == "f":
                                kb.op("act", lambda e: e.activation(out=bufB[:, sl_], in_=PS[bk][:], func=AF.Sigmoid),
                                      reads=[PR[bk]], writes=[rB])
                            else:
                                kb.op("act", lambda e: e.activation(out=sg[:, sl_], in_=PS[bk][:], func=AF.Silu),
                                      reads=[PR[bk]], writes=[r_sg])
                    for grp in range(4):
                        bk = 4
                        for j in range(4):
                            tt = grp * 4 + j
                            for c in range(16):
                                kb.op("pe", lambda e: e.matmul(PS[bk][:, j * 128:(j + 1) * 128],
                                                               lhsT=xnT[:, c, tt * 128:(tt + 1) * 128],
                                                               rhs=wl[:, c, 256:384],
                                                               start=(c == 0), stop=(c == 15)),
                                      reads=[r_wl, r_xnT], writes=[PR[bk]])
                        kb.op("dve", lambda e: e.tensor_copy(out=vtok[:, grp * 4:(grp + 1) * 4, :],
                                                             in_=PS[bk][:].rearrange("p (j v) -> p j v", j=4)),
                              reads=[PR[bk]], writes=[r_vtok])
                    kb.op("dve", lambda e: e.tensor_scalar(out=bufB[:], in0=bufB[:], scalar1=lbt[:, h, 3:4],
                                                           scalar2=lbt[:, h, 2:3], op0=ALU.mult, op1=ALU.add),
                          reads=[rB, r_lbt], writes=[rB])
                    kb.op("act", lambda e: e.activation(out=bufC[:], in_=bufB[:], func=AF.Ln),
                          reads=[rB], writes=[rC])
                    kb.op("dve", lambda e: e.tensor_tensor_scan(out=bufD[:], data0=rmask[:], data1=bufC[:],
                                                                initial=0.0, op0=ALU.mult, op1=ALU.add),
                          reads=[rC, r_rmask], writes=[rD])
                    kb.op("act", lambda e: e.activation(out=bufC[:], in_=bufD[:], func=AF.Exp),
                          reads=[rD], writes=[rC])
                    kb.op("act", lambda e: e.activation(out=bufD[:], in_=bufD[:], func=AF.Exp, scale=-1.0),
                          reads=[rD], writes=[rD])
                    kb.op("dve", lambda e: e.tensor_tensor(out=qd[:], in0=bufA[:], in1=bufC[:], op=ALU.mult),
                          reads=[rA, rC], writes=[r_qd])
                    kb.op("pool", lambda e: e.tensor_copy(
                        out=dec[:], in_=bufC[:].rearrange("p (n c) -> p n c", c=128)[:, :, 127]),
                          reads=[rC], writes=[r_dec])
                    kb.op("dve", lambda e: e.tensor_scalar(out=bufB[:], in0=bufB[:], scalar1=-1.0, scalar2=1.0,
                                                           op0=ALU.mult, op1=ALU.add),
                          reads=[rB], writes=[rB])
                    kb.op("dve", lambda e: e.tensor_tensor(out=kd[:], in0=bufB[:], in1=bufD[:], op=ALU.mult),
                          reads=[rB, rD], writes=[r_kd])
                    for half in range(2):
                        bk = 5
                        pv = PS[bk][:].bitcast(BF16)
                        for j in range(8):
                            n = half * 8 + j
                            kb.op("pe", lambda e: e.transpose(out=pv[:, j * 128:(j + 1) * 128],
                                                              in_=kd[:, n * 128:(n + 1) * 128], identity=ident_b[:]),
                                  reads=[r_kd, r_const], writes=[PR[bk]])
                        kb.op("dve", lambda e: e.tensor_copy(out=kdtok[:, half * 8:(half + 1) * 8, :],
                                                             in_=pv.rearrange("p (j c) -> p j c", j=8)),
                              reads=[PR[bk]], writes=[r_kdtok])
                    if h == 0:
                        dump("xnT0", xnT[:, 0, :], [128, T], BF16, [r_xnT])
                        dump("xnT15", xnT[:, 15, :], [128, T], BF16, [r_xnT])
                        dump("wl", wl[:, 0, :], [128, 512], BF16, [r_wl])
                        dump("q", bufA[:], [128, T], F32, [rA])
                        dump("k", bufB[:], [128, T], F32, [rB])
                        dump("eb", bufC[:], [128, T], F32, [rC])
                        dump("emb", bufD[:], [128, T], F32, [rD])
                        dump("qd", qd[:], [128, T], BF16, [r_qd])
                        dump("kd", kd[:], [128, T], BF16, [r_kd])
                        dump("sg", sg[:], [128, T], BF16, [r_sg])
                        dump("vtok", vtok[:].rearrange("p a b -> p (a b)"), [128, T], BF16, [r_vtok])
                        dump("kdtok", kdtok[:].rearrange("p a b -> p (a b)"), [128, T], BF16, [r_kdtok])
                        dump("dec", dec[:], [128, 16], F32, [r_dec])
                        dump("lbt", lbt[:].rearrange("p a b -> p (a b)"), [128, 32], F32, [r_lbt])
                    os_ = h % 2
                    for n in range(16):
                        a_ = n % 2
                        tsl = slice(n * 128, (n + 1) * 128)
                        pA = PS[6][:, 0:128]
                        kb.op("pe", lambda e: e.matmul(pA, lhsT=kd[:, tsl], rhs=qd[:, tsl], start=True, stop=True),
                              reads=[r_kd, r_qd], writes=[PR[6]])
                        kb.op("dve", lambda e: e.tensor_tensor(out=at[a_][:], in0=pA, in1=triLE_b[:], op=ALU.mult),
                              reads=[PR[6], r_const], writes=[r_at[a_]])
                        pO = PS[7][:, 0:128]
                        kb.op("pe", lambda e: e.matmul(pO, lhsT=vtok[:, n, :], rhs=at[a_][:], start=True,
                                                       stop=(n == 0)),
                              reads=[r_vtok, r_at[a_]], writes=[PR[7]])
                        if n > 0:
                            kb.op("pe", lambda e: e.matmul(pO, lhsT=Sbf[:], rhs=qd[:, tsl], start=False, stop=True),
                                  reads=[r_Sbf, r_qd], writes=[PR[7]])
                        if n < 15:
                            pU = PS[4][:, 0:128]
                            kb.op("pe", lambda e: e.matmul(pU, lhsT=kdtok[:, n, :], rhs=vtok[:, n, :],
                                                           start=True, stop=True),
                                  reads=[r_kdtok, r_vtok], writes=[PR[4]])
                            if n == 0:
                                kb.op("dve", lambda e: e.tensor_copy(out=Stmp[:], in_=pU),
                                      reads=[PR[4]], writes=[r_Stmp])
                            else:
                                kb.op("dve", lambda e: e.tensor_tensor(out=Stmp[:], in0=pU, in1=S32[:], op=ALU.add),
                                      reads=[PR[4], r_S32], writes=[r_Stmp])
                            kb.op("dve", lambda e: e.tensor_scalar(out=S32[:], in0=Stmp[:], scalar1=dec[:, n:n + 1],
                                                                   scalar2=None, op0=ALU.mult),
                                  reads=[r_Stmp, r_dec], writes=[r_S32])
                            kb.op("act", lambda e: e.activation(out=Sbf[:], in_=Stmp[:], func=AF.Copy,
                                                                scale=dec[:, n:n + 1]),
                                  reads=[r_Stmp, r_dec], writes=[r_Sbf])
                        kb.op("act", lambda e: e.activation(out=sq[a_][:], in_=pO, func=AF.Square),
                              reads=[PR[7]], writes=[r_sq[a_]])
                        pS = PS[5][:, 0:128]
                        kb.op("pe", lambda e: e.matmul(pS, lhsT=ones_b[:], rhs=sq[a_][:], start=True, stop=True),
                              reads=[r_sq[a_], r_const], writes=[PR[5]])
                        kb.op("act", lambda e: e.activation(out=lnv[a_][:], in_=pS, func=AF.Ln, scale=1.0 / 128,
                                                            bias=EPS),
                              reads=[PR[5]], writes=[r_lnv[a_]])
                        kb.op("act", lambda e: e.activation(out=lnv[a_][:], in_=lnv[a_][:], func=AF.Exp, scale=-0.5),
                              reads=[r_lnv[a_]], writes=[r_lnv[a_]])
                        kb.op("dve", lambda e: e.tensor_tensor(out=t1[a_][:], in0=pO, in1=lnv[a_][:], op=ALU.mult),
                              reads=[PR[7], r_lnv[a_]], writes=[r_t1[a_]])
                        kb.op("dve", lambda e: e.scalar_tensor_tensor(out=ost[os_][:, tsl], in0=t1[a_][:],
                                                                      scalar=hgw[:, 0:1], in1=sg[:, tsl],
                                                                      op0=ALU.mult, op1=ALU.mult),
                              reads=[r_t1[a_], r_sg, r_lbt], writes=[r_ost[os_]])
                    kb.dma("sp", r_ost[os_], lambda e: e.dma_start(out=omix[h * 128:(h + 1) * 128, :], in_=ost[os_][:]),
                           reads=[r_ost[os_]], writes=[r_omix])
                    if h == 0:
                        dump("ost", ost[os_][:], [128, T], BF16, [r_ost[os_]])
                        dump("S32", S32[:], [128, 128], F32, [r_S32])
                        dump("t1", t1[1][:], [128, 128], F32, [r_t1[1]])
                        dump("lnv", lnv[1][:], [128, 128], F32, [r_lnv[1]])
                        dump("at", at[1][:], [128, 128], BF16, [r_at[1]])

            kb.barrier()
            with contextlib.ExitStack() as pc_:
                slopeR = sb(pc_, "slopeR", [9, 16 * 128], BF16)
                posL = sb(pc_, "posL", [9, 16 * 128], BF16)
                posC = sb(pc_, "posC", [9, 16 * 128], BF16)
                maskC = sb(pc_, "maskC", [128, 16 * 128], BF16)
                Esel = sb(pc_, "Esel", [32, 16 * 128], BF16)
                M1 = sb(pc_, "M1", [128, 512], F32)
                M2 = sb(pc_, "M2", [128, 512], F32)
                cload(slopeR[:], tabs["slopeR"][:, :], True)
                cload(posL[:], tabs["posL"][:, :], True)
                cload(posC[:], tabs["posC"][:, :], True)
                cload(maskC[:], tabs["maskC"][:, :], True)
                cload(Esel[:], tabs["Esel"][:, :], True)
                cload(M1[:], tabs["M1"][:, :], False)
                cload(M2[:], tabs["M2"][:, :], False)
                nw = sb(pc_, "nw", [128, 4], F32)
                kb.op("dve", lambda e: e.memset(nw[:], 0.0), writes=[r_const])
                for (col, src, reps) in ((0, q_norm_w, 2), (1, kc_norm_w, 1), (2, ks_norm_w, 1), (3, kw_norm_w, 1)):
                    for rp in range(reps):
                        kb.dma("sp", r_const, lambda e: e.dma_start(out=nw[rp * 64:(rp + 1) * 64, col:col + 1],
                                                                    in_=src.rearrange("o p -> p o"),
                                                                    allow_slow_non_contiguous=True),
                               writes=[r_const])
                kcn = [sb(pc_, "kcn%d" % i, [128, 4, 128], BF16) for i in range(2)]
                r_kcn = R("kcn")
                vcmp = sb(pc_, "vcmp", [128, 4, 98], BF16)
                r_vcmp = R("vcmp")
                gts = sb(pc_, "gts", [128, 16, 48], F32)
                r_gts = R("gts")
                kb.op("pool", lambda e: e.memset(kcn[1][:], 0.0), writes=[r_kcn])
                kb.op("pool", lambda e: e.memset(vcmp[:], 1.0), writes=[r_vcmp])
                for g in range(4):
                    kb.dma("pool", r_vcmp, lambda e: e.dma_start(out=vcmp[:, g, 65:97], in_=tabs["ovl"][:, :]),
                           writes=[r_vcmp])

                wl, r_wl = load_wcols([(C_GT, 48)])
                for tt in range(NT):
                    bk = tt % 2
                    for c in range(16):
                        kb.op("pe", lambda e: e.matmul(PS[bk][:, 0:48], lhsT=xnT[:, c, tt * 128:(tt + 1) * 128],
                                                       rhs=wl[:, c, 0:48], start=(c == 0), stop=(c == 15)),
                              reads=[r_wl, r_xnT], writes=[PR[bk]])
                    kb.op("act", lambda e: e.activation(out=gts[:, tt, :], in_=PS[bk][:, 0:48], func=AF.Sigmoid),
                          reads=[PR[bk]], writes=[r_gts])

                with contextlib.ExitStack() as pc1:
                    kvc = sb(pc1, "kvc", [128, 4, T], BF16)
                    r_kvc = R("kvc")
                    w1 = sb(pc1, "w1c", [128, 32, 256], BF16)
                    w2 = sb(pc1, "w2c", [128, 2, 2, 128], BF16)
                    posT = sb(pc1, "posT", [128, 32], BF16)
                    r_cw = R("cw")
                    kb.op("pool", lambda e: e.memset(w2[:], 0.0), writes=[r_cw])
                    for wh, (wa, wb, pp) in enumerate(((w_ck1, w_ck2, cmp_pos_k), (w_cv1, w_cv2, cmp_pos_v))):
                        kb.dma("pool", r_cw, lambda e: e.dma_start(
                            out=w1[wh * 64:(wh + 1) * 64, :, :],
                            in_=wa[0].rearrange("(l d) j -> d l j", d=64)), writes=[r_cw])
                        kb.dma("pool", r_cw, lambda e: e.dma_start(
                            out=w2[:, wh, :, 0:64],
                            in_=wb[0].rearrange("(jb j) d -> j jb d", j=128)), writes=[r_cw])
                        kb.dma("pool", r_cw, lambda e: e.dma_start(
                            out=posT[wh * 64:(wh + 1) * 64, :],
                            in_=pp[0].rearrange("l d -> d l"), allow_slow_non_contiguous=True), writes=[r_cw])
                    wl, r_wl = load_wcols([(C_KC, 512)])
                    wkv = sb(pc1, "wkv", [128, 16, 128], BF16)
                    r_wkv = R("wkv")
                    for g in range(4):
                        kb.op("dve", lambda e: e.tensor_copy(out=wkv[:, :, 0:64], in_=wl[:, :, g * 64:(g + 1) * 64]),
                              reads=[r_wl], writes=[r_wkv])
                        kb.op("dve", lambda e: e.tensor_copy(out=wkv[:, :, 64:128],
                                                             in_=wl[:, :, 256 + g * 64:256 + (g + 1) * 64]),
                              reads=[r_wl], writes=[r_wkv])
                        for tb_ in range(4):
                            bk = tb_ % 2
                            for c in range(16):
                                kb.op("pe", lambda e: e.matmul(PS[bk][:], lhsT=wkv[:, c, :],
                                                               rhs=xnT[:, c, tb_ * 512:(tb_ + 1) * 512],
                                                               start=(c == 0), stop=(c == 15)),
                                      reads=[r_wkv, r_xnT], writes=[PR[bk]])
                            kb.op("act", lambda e: e.copy(out=kvc[:, g, tb_ * 512:(tb_ + 1) * 512], in_=PS[bk][:]),
                                  reads=[PR[bk]], writes=[r_kvc])
                    cb = sb(pc1, "cb", [128, 2, 2], F32)
                    r_cb = R("cb")
                    for wh in range(2):
                        ps_ = slice(wh * 64, (wh + 1) * 64)
                        for jb in range(2):
                            bk = 2
                            for l in range(32):
                                kb.op("pe", lambda e: e.matmul(PS[bk][:, 0:1], lhsT=w1[ps_, l, jb * 128:(jb + 1) * 128],
                                                               rhs=posT[ps_, l:l + 1], start=(l == 0), stop=(l == 31)),
                                      reads=[r_cw], writes=[PR[bk]])
                            kb.op("dve", lambda e: e.tensor_copy(out=cb[:, wh, jb:jb + 1], in_=PS[bk][:, 0:1]),
                                  reads=[PR[bk]], writes=[r_cb])
                    xh = sb(pc1, "xh", [128, 128], F32)
                    uu = sb(pc1, "uu", [128, 128], F32)
                    hT = sb(pc1, "hT", [128, 2, 128], BF16)
                    r_xh, r_uu, r_hT = R("xh"), R("uu"), R("hT")
                    sqc = sb(pc1, "sqc", [128, 128], BF16)
                    lnc = sb(pc1, "lnc", [128, 128], F32)
                    r_sqc, r_lnc = R("sqc"), R("lnc")
                    for g in range(4):
                        for wh in range(2):
                            ps_ = slice(wh * 64, (wh + 1) * 64)
                            for jb in range(2):
                                bk = 3
                                for l in range(32):
                                    kb.op("pe", lambda e: e.matmul(
                                        PS[bk][:, 0:127], lhsT=w1[ps_, l, jb * 128:(jb + 1) * 128],
                                        rhs=kvc[ps_, g, l:l + 16 * 126 + 1:16], start=(l == 0), stop=(l == 31)),
                                          reads=[r_cw, r_kvc], writes=[PR[bk]])
                                kb.op("act", lambda e: e.activation(out=xh[:, 0:127], in_=PS[bk][:, 0:127],
                                                                    func=AF.Identity, bias=cb[:, wh, jb:jb + 1]),
                                      reads=[PR[bk], r_cb], writes=[r_xh])
                                kb.op("dve", lambda e: e.tensor_tensor(out=uu[:, 0:127], in0=xh[:, 0:127],
                                                                       in1=xh[:, 0:127], op=ALU.mult),
                                      reads=[r_xh], writes=[r_uu])
                                kb.op("dve", lambda e: e.tensor_scalar(out=uu[:, 0:127], in0=uu[:, 0:127],
                                                                       scalar1=0.044715, scalar2=1.0,
                                                                       op0=ALU.mult, op1=ALU.add),
                                      reads=[r_uu], writes=[r_uu])
                                kb.op("dve", lambda e: e.tensor_tensor(out=uu[:, 0:127], in0=uu[:, 0:127],
                                                                       in1=xh[:, 0:127], op=ALU.mult),
                                      reads=[r_uu, r_xh], writes=[r_uu])
                                kb.op("act", lambda e: e.activation(out=uu[:, 0:127], in_=uu[:, 0:127],
                                                                    func=AF.Sigmoid, scale=1.5957691216),
                                      reads=[r_uu], writes=[r_uu])
                                kb.op("dve", lambda e: e.tensor_tensor(out=hT[:, jb, 0:127], in0=uu[:, 0:127],
                                                                       in1=xh[:, 0:127], op=ALU.mult),
                                      reads=[r_uu, r_xh], writes=[r_hT])
                            if wh == 0:
                                bk = 2
                                for jb in range(2):
                                    kb.op("pe", lambda e: e.matmul(PS[bk][:, 0:127], lhsT=w2[:, 0, jb, :],
                                                                   rhs=hT[:, jb, 0:127], start=(jb == 0), stop=(jb == 1)),
                                          reads=[r_cw, r_hT], writes=[PR[bk]])
                                kb.op("act", lambda e: e.activation(out=sqc[:, 0:127], in_=PS[bk][:, 0:127],
                                                                    func=AF.Square),
                                      reads=[PR[bk]], writes=[r_sqc])
                                kb.op("pe", lambda e: e.matmul(PS[bk][:, 128:255], lhsT=ones_b[:], rhs=sqc[:, 0:127],
                                                               start=True, stop=True),
                                      reads=[r_sqc, r_const], writes=[PR[bk]])
                                kb.op("act", lambda e: e.activation(out=lnc[:, 0:127], in_=PS[bk][:, 128:255],
                                                                    func=AF.Ln, scale=1.0 / 64, bias=EPS),
                                      reads=[PR[bk]], writes=[r_lnc])
                                kb.op("act", lambda e: e.activation(out=lnc[:, 0:127], in_=lnc[:, 0:127],
                                                                    func=AF.Exp, scale=-0.5),
                                      reads=[r_lnc], writes=[r_lnc])
                                kb.op("dve", lambda e: e.scalar_tensor_tensor(
                                    out=kcn[0][:, g, 0:127], in0=PS[bk][:, 0:127], scalar=nw[:, 1:2],
                                    in1=lnc[:, 0:127], op0=ALU.mult, op1=ALU.mult),
                                      reads=[PR[bk], r_lnc, r_const], writes=[r_kcn])
                            else:
                                bk = 2
                                for jb in range(2):
                                    kb.op("pe", lambda e: e.matmul(PS[bk][0:127, 0:64], lhsT=hT[:, jb, 0:127],
                                                                   rhs=w2[:, 1, jb, 0:64], start=(jb == 0), stop=(jb == 1)),
                                          reads=[r_cw, r_hT], writes=[PR[bk]])
                                kb.op("dve", lambda e: e.tensor_copy(out=vcmp[0:127, g, 0:64], in_=PS[bk][0:127, 0:64]),
                                      reads=[PR[bk]], writes=[r_vcmp])
                    kb.dma("sp", r_kcn, lambda e: e.dma_start(out=kcn[1][64:128, :, :], in_=kcn[0][0:64, :, :]),
                           reads=[r_kcn], writes=[r_kcn])

                kb.barrier()
                qn = sb(pc_, "qn", [128, 2, T], BF16)
                r_qn = R("qn")
                kS = [sb(pc_, "kS%d" % i, [128, T], BF16) for i in range(2)]
                kW = [sb(pc_, "kW%d" % i, [128, T], BF16) for i in range(2)]
                r_kS, r_kW = R("kS"), R("kW")
                vS = sb(pc_, "vS", [128, 16, 66], BF16)
                vW = sb(pc_, "vW", [128, 16, 66], BF16)
                r_vS, r_vW = R("vS"), R("vW")
                wpad = sb(pc_, "wpad", [128, 16, 128], BF16)
                r_wpad = R("wpad")
                kb.op("pool", lambda e: e.memset(wpad[:], 0.0), writes=[r_wpad])
                sqn = [sb(pc_, "sqn%d" % i, [128, 512], BF16) for i in range(2)]
                r_sqn = [R("sqn%d" % i) for i in range(2)]
                lnn = [sb(pc_, "lnn%d" % i, [128, 512], F32) for i in range(2)]
                r_lnn = [R("lnn%d" % i) for i in range(2)]
                pT = [sb(pc_, "pT%d" % i, [128, 512], BF16) for i in range(3)]
                r_pT = [R("pT%d" % i) for i in range(3)]
                oacc = [sb(pc_, "oacc%d" % i, [128, 256], F32) for i in range(2)]
                r_oacc = [R("oacc%d" % i) for i in range(2)]
                sm = sb(pc_, "sm", [128, 2, 16], F32)
                r_sm = [R("sm0"), R("sm1")]
                imp = sb(pc_, "imp", [128, 2, 3, 32], F32)
                r_imp = [R("imp0"), R("imp1")]
                mx = sb(pc_, "mx", [128, 2, 16], F32)
                selb = sb(pc_, "selb", [128, 2, 32], F32)
                selT = [sb(pc_, "selT%d" % i, [32, 512], BF16) for i in range(2)]
                r_selT = [R("selT0"), R("selT1")]
                ostn = [sb(pc_, "ostn%d" % i, [128, 2, 128], BF16) for i in range(2)]
                r_ostn = [R("ostn0"), R("ostn1")]
                for g in range(NG):
                    kb.op("dve", lambda e: e.memset(kS[1][:], 0.0), writes=[r_kS])
                    kb.op("dve", lambda e: e.memset(kW[1][:], 0.0), writes=[r_kW])
                    kb.op("pool", lambda e: e.memset(vS[:], 1.0), writes=[r_vS])
                    kb.op("pool", lambda e: e.memset(vW[:], 1.0), writes=[r_vW])
                    wl, r_wl = load_wcols([(C_NQ + g * 256, 256), (C_KS + g * 64, 64), (C_VS + g * 64, 64),
                                           (C_KW + g * 64, 64), (C_VW + g * 64, 64)])
                    bkc = 0
                    ncnt = 0
                    for which in range(4):
                        if which >= 2:
                            cs = 256 + (which - 2) * 128
                            kb.op("dve", lambda e: e.tensor_copy(out=wpad[:, :, 0:64], in_=wl[:, :, cs:cs + 64]),
                                  reads=[r_wl], writes=[r_wpad])
                        for tb_ in range(4):
                            bk = bkc % 2
                            bkc += 1
                            for c in range(16):
                                if which < 2:
                                    lh = wl[:, c, which * 128:(which + 1) * 128]
                                    rr = [r_wl, r_xnT]
                                else:
                                    lh = wpad[:, c, :]
                                    rr = [r_wpad, r_xnT]
                                kb.op("pe", lambda e: e.matmul(PS[bk][:], lhsT=lh,
                                                               rhs=xnT[:, c, tb_ * 512:(tb_ + 1) * 512],
                                                               start=(c == 0), stop=(c == 15)),
                                      reads=rr, writes=[PR[bk]])
                            n_ = ncnt % 2
                            ncnt += 1
                            kb.op("act", lambda e: e.activation(out=sqn[n_][:], in_=PS[bk][:], func=AF.Square),
                                  reads=[PR[bk]], writes=[r_sqn[n_]])
                            b2 = 2 + bk
                            ones_ = onesblk_b if which < 2 else ones_b
                            kb.op("pe", lambda e: e.matmul(PS[b2][:], lhsT=ones_[:], rhs=sqn[n_][:],
                                                           start=True, stop=True),
                                  reads=[r_sqn[n_], r_const], writes=[PR[b2]])
                            kb.op("act", lambda e: e.activation(out=lnn[n_][:], in_=PS[b2][:], func=AF.Ln,
                                                                scale=1.0 / 64, bias=EPS),
                                  reads=[PR[b2]], writes=[r_lnn[n_]])
                            kb.op("act", lambda e: e.activation(out=lnn[n_][:], in_=lnn[n_][:], func=AF.Exp,
                                                                scale=-0.5,
                                                                bias=(float(np.log(0.125)) if which < 2 else 0.0)),
                                  reads=[r_lnn[n_]], writes=[r_lnn[n_]])
                            sl_ = slice(tb_ * 512, (tb_ + 1) * 512)
                            if which < 2:
                                dst, rd, wc = qn[:, which, sl_], r_qn, 0
                            elif which == 2:
                                dst, rd, wc = kS[0][:, sl_], r_kS, 2
                            else:
                                dst, rd, wc = kW[0][:, sl_], r_kW, 3
                            kb.op("dve", lambda e: e.scalar_tensor_tensor(out=dst, in0=PS[bk][:], scalar=nw[:, wc:wc + 1],
                                                                          in1=lnn[n_][:], op0=ALU.mult, op1=ALU.mult),
                                  reads=[PR[bk], r_lnn[n_], r_const], writes=[rd])
                    kb.dma("sp", r_kS, lambda e: e.dma_start(out=kS[1][64:128, :], in_=kS[0][0:64, :]),
                           reads=[r_kS], writes=[r_kS])
                    kb.dma("sp", r_kW, lambda e: e.dma_start(out=kW[1][64:128, :], in_=kW[0][0:64, :]),
                           reads=[r_kW], writes=[r_kW])
                    for tt in range(NT):
                        bk = 4 + tt % 2
                        for c in range(16):
                            kb.op("pe", lambda e: e.matmul(PS[bk][:, 0:256], lhsT=xnT[:, c, tt * 128:(tt + 1) * 128],
                                                           rhs=wl[:, c, 256:512], start=(c == 0), stop=(c == 15)),
                                  reads=[r_wl, r_xnT], writes=[PR[bk]])
                        kb.op("act", lambda e: e.copy(out=vS[:, tt, 0:64], in_=PS[bk][:, 64:128]),
                              reads=[PR[bk]], writes=[r_vS])
                        kb.op("act", lambda e: e.copy(out=vW[:, tt, 0:64], in_=PS[bk][:, 192:256]),
                              reads=[PR[bk]], writes=[r_vW])

                    if g == 0:
                        dump("qn", qn[:].rearrange("p a t -> p (a t)"), [128, 2 * T], BF16, [r_qn])
                        dump("kS0", kS[0][:], [128, T], BF16, [r_kS])
                        dump("kS1", kS[1][:], [128, T], BF16, [r_kS])
                        dump("kW0", kW[0][:], [128, T], BF16, [r_kW])
                        dump("vS", vS[:].rearrange("p a b -> p (a b)"), [128, 16 * 66], BF16, [r_vS])
                        dump("vW", vW[:].rearrange("p a b -> p (a b)"), [128, 16 * 66], BF16, [r_vW])
                        dump("kcn0", kcn[0][:].rearrange("p a b -> p (a b)"), [128, 512], BF16, [r_kcn])
                        dump("kcn1", kcn[1][:].rearrange("p a b -> p (a b)"), [128, 512], BF16, [r_kcn])
                        dump("vcmp", vcmp[:].rearrange("p a b -> p (a b)"), [128, 4 * 98], BF16, [r_vcmp])
                        dump("gts", gts[:].rearrange("p a b -> p (a b)"), [128, 16 * 48], F32, [r_gts])
                    scnt = [0]
                    ocnt = [0]

                    def bank(tt, st, branch):
                        sbk = scnt[0] % 3
                        scnt[0] += 1
                        psb = PS[sbk]
                        rps = PR[sbk]
                        p_ = pT[sbk]
                        rp_ = r_pT[sbk]
                        if branch == 0:
                            nk = min(127, 8 * tt + 7)
                            lpos = posC[0:9, tt * 128:tt * 128 + nk]
                        else:
                            nk = 128
                            lpos = posL[0:9, (tt - st) * 128:(tt - st + 1) * 128]
                        kb.op("pe", lambda e: e.matmul(psb[0:nk, :], lhsT=lpos,
                                                       rhs=slopeR[0:9, g * 512:(g + 1) * 512], start=True, stop=False),
                              reads=[r_const], writes=[rps])
                        if branch == 1:
                            si = tt % 2
                            kb.op("pe", lambda e: e.matmul(
                                psb[:, :], lhsT=Esel[0:32, st * 128:(st + 1) * 128],
                                rhs=selT[si][:, :], start=False, stop=False),
                                  reads=[r_const, r_selT[si]], writes=[rps])
                        for r in range(4):
                            if branch == 0:
                                lh = kcn[r % 2][:, g, 0:nk]
                                rk = r_kcn
                            elif branch == 1:
                                lh = kS[r % 2][:, st * 128:(st + 1) * 128]
                                rk = r_kS
                            else:
                                lh = kW[r % 2][:, st * 128:(st + 1) * 128]
                                rk = r_kW
                            kb.op("pe", lambda e: e.matmul(psb[0:nk, r * 128:(r + 1) * 128], lhsT=lh,
                                                           rhs=qn[:, r // 2, tt * 128:(tt + 1) * 128],
                                                           start=False, stop=(r == 3)),
                                  reads=[rk, r_qn], writes=[rps])
                        kb.op("act", lambda e: e.activation(out=p_[0:nk, :], in_=psb[0:nk, :], func=AF.Exp),
                              reads=[rps], writes=[rp_])
                        msk = None
                        if branch == 0:
                            msk = maskC[0:nk, tt * 128:(tt + 1) * 128]
                        elif st == tt:
                            msk = triLE_b[:, :]
                        elif branch == 2 and st == tt - 4:
                            msk = triGT_b[:, :]
                        if msk is not None:
                            kb.op("dve", lambda e: e.tensor_tensor(
                                out=p_[0:nk, :].rearrange("p (r t) -> p r t", r=4),
                                in0=p_[0:nk, :].rearrange("p (r t) -> p r t", r=4),
                                in1=msk.unsqueeze(1).to_broadcast([nk, 4, 128]), op=ALU.mult),
                                  reads=[rp_, r_const], writes=[rp_])
                        return p_, rp_, nk

                    def finalize(pso, rpo, tt, branch, width, oa, r_oa, ii):
                        if dumps and g == 0 and tt == 3:
                            dscr = sb(pc_, "dscr%d" % branch, [128, 4, 98], F32)
                            r_dscr = R("dscr%d" % branch)
                            kb.op("dve", lambda e: e.tensor_copy(out=dscr[:, :, 0:width], in_=pso[:, :, 0:width]),
                                  reads=[rpo], writes=[r_dscr])
                            dump("pso%d" % branch, dscr[:].rearrange("p a b -> p (a b)"), [128, 392], F32, [r_dscr])
                        kb.op("dve", lambda e: e.tensor_scalar(out=sm[:, ii, 0:4], in0=pso[:, :, 64], scalar1=1e-30,
                                                               scalar2=None, op0=ALU.add),
                              reads=[rpo], writes=[r_sm[ii]])
                        kb.op("dve", lambda e: e.reciprocal(out=sm[:, ii, 4:8], in_=sm[:, ii, 0:4]),
                              reads=[r_sm[ii]], writes=[r_sm[ii]])
                        if branch == 0:
                            for r in range(4):
                                if r == 0:
                                    kb.op("dve", lambda e: e.tensor_scalar(out=imp[:, ii, 0, :], in0=pso[:, 0, 65:97],
                                                                           scalar1=sm[:, ii, 4:5], scalar2=None,
                                                                           op0=ALU.mult),
                                          reads=[rpo, r_sm[ii]], writes=[r_imp[ii]])
                                else:
                                    kb.op("dve", lambda e: e.scalar_tensor_tensor(
                                        out=imp[:, ii, 0, :], in0=pso[:, r, 65:97], scalar=sm[:, ii, 4 + r:5 + r],
                                        in1=imp[:, ii, 0, :], op0=ALU.mult, op1=ALU.add),
                                          reads=[rpo, r_sm[ii], r_imp[ii]], writes=[r_imp[ii]])
                        gv = gts[:, tt, g * 12:(g + 1) * 12].rearrange("p (r b) -> p r b", b=3)[:, :, branch]
                        kb.op("dve", lambda e: e.tensor_tensor(out=sm[:, ii, 8:12], in0=sm[:, ii, 4:8], in1=gv,
                                                               op=ALU.mult),
                              reads=[r_sm[ii], r_gts], writes=[r_sm[ii]])
                        for r in range(4):
                            if branch == 0:
                                kb.op("dve", lambda e: e.tensor_scalar(out=oa[:, r * 64:(r + 1) * 64], in0=pso[:, r, 0:64],
                                                                       scalar1=sm[:, ii, 8 + r:9 + r], scalar2=None,
                                                                       op0=ALU.mult),
                                      reads=[rpo, r_sm[ii]], writes=[r_oa])
                            else:
                                kb.op("dve", lambda e: e.scalar_tensor_tensor(
                                    out=oa[:, r * 64:(r + 1) * 64], in0=pso[:, r, 0:64], scalar=sm[:, ii, 8 + r:9 + r],
                                    in1=oa[:, r * 64:(r + 1) * 64], op0=ALU.mult, op1=ALU.add),
                                      reads=[rpo, r_sm[ii], r_oa], writes=[r_oa])

                    for tt in range(NT):
                        ii = tt % 2
                        oa, r_oa = oacc[ii], r_oacc[ii]
                        p_, rp_, nk = bank(tt, 0, 0)
                        obk = 3 + ocnt[0] % 2
                        ocnt[0] += 1
                        pso = PS[obk][:, 0:392].rearrange("p (r w) -> p r w", r=4)
                        for r in range(4):
                            kb.op("pe", lambda e: e.matmul(pso[:, r, 0:97], lhsT=p_[0:nk, r * 128:(r + 1) * 128],
                                                           rhs=vcmp[0:nk, g, 0:97], start=True, stop=True),
                                  reads=[rp_, r_vcmp], writes=[PR[obk]])
                        if g == 0 and tt == 3:
                            dump("pTc", p_[:], [128, 512], BF16, [rp_])
                        finalize(pso, PR[obk], tt, 0, 97, oa, r_oa, ii)
                        if g == 0 and tt == 3:
                            dump("sm0", sm[:].rearrange("p a b -> p (a b)"), [128, 32], F32, r_sm)
                        kb.op("dve", lambda e: e.tensor_tensor(out=imp[:, ii, 1, :], in0=imp[:, ii, 0, :],
                                                               in1=M1[:, tt * 32:(tt + 1) * 32], op=ALU.mult),
                              reads=[r_imp[ii], r_const], writes=[r_imp[ii]])
                        kb.op("dve", lambda e: e.tensor_tensor(out=imp[:, ii, 1, :], in0=imp[:, ii, 1, :],
                                                               in1=M2[:, tt * 32:(tt + 1) * 32], op=ALU.add),
                              reads=[r_imp[ii], r_const], writes=[r_imp[ii]])
                        kb.op("dve", lambda e: e.max(out=mx[:, ii, 0:8], in_=imp[:, ii, 1, :]),
                              reads=[r_imp[ii]], writes=[r_imp[ii]])
                        kb.op("dve", lambda e: e.match_replace(out=imp[:, ii, 2, :], in_to_replace=mx[:, ii, 0:8],
                                                               in_values=imp[:, ii, 1, :], imm_value=-3.0e38),
                              reads=[r_imp[ii]], writes=[r_imp[ii]])
                        kb.op("dve", lambda e: e.max(out=mx[:, ii, 8:16], in_=imp[:, ii, 2, :]),
                              reads=[r_imp[ii]], writes=[r_imp[ii]])
                        kb.op("dve", lambda e: e.tensor_scalar(out=selb[:, ii, :], in0=imp[:, ii, 1, :],
                                                               scalar1=mx[:, ii, 15:16], scalar2=NEG,
                                                               op0=ALU.is_lt, op1=ALU.mult),
                              reads=[r_imp[ii]], writes=[r_imp[ii]])
                        kb.op("pe", lambda e: e.transpose(out=PS[5][0:32, 0:128], in_=selb[:, ii, :],
                                                          identity=ident_f[:]),
                              reads=[r_imp[ii], r_const], writes=[PR[5]])
                        kb.op("act", lambda e: e.copy(
                            out=selT[ii][:, :].rearrange("p (r t) -> p r t", r=4),
                            in_=PS[5][0:32, 0:128].unsqueeze(1).to_broadcast([32, 4, 128])),
                              reads=[PR[5]], writes=[r_selT[ii]])
                        for branch in (1, 2):
                            obk = 3 + ocnt[0] % 2
                            ocnt[0] += 1
                            pso = PS[obk][:, 0:264].rearrange("p (r w) -> p r w", r=4)
                            kb.op("pe", lambda e: e.matmul(PS[obk][:, 0:264], lhsT=zrow[0:1, 0:128],
                                                           rhs=zrow[0:1, 0:264], start=True, stop=False),
                                  reads=[r_const], writes=[PR[obk]])
                            st0 = 0 if branch == 1 else max(0, tt - 4)
                            vt, rv = (vS, r_vS) if branch == 1 else (vW, r_vW)
                            for st in range(st0, tt + 1):
                                p_, rp_, nk = bank(tt, st, branch)
                                for r in range(4):
                                    kb.op("pe", lambda e: e.matmul(pso[:, r, 0:65], lhsT=p_[:, r * 128:(r + 1) * 128],
                                                                   rhs=vt[:, st, 0:65], start=False,
                                                                   stop=(st == tt and r == 3)),
                                          reads=[rp_, rv], writes=[PR[obk]])
                            finalize(pso, PR[obk], tt, branch, 66, oa, r_oa, ii)
                        for hf in range(2):
                            kb.op("pe", lambda e: e.transpose(out=PS[6][:, hf * 128:(hf + 1) * 128],
                                                              in_=oa[:, hf * 128:(hf + 1) * 128], identity=ident_f[:]),
                                  reads=[r_oa, r_const], writes=[PR[6]])
                        kb.op("act", lambda e: e.copy(out=ostn[ii][:, :, :],
                                                      in_=PS[6][:, 0:256].rearrange("p (a t) -> p a t", a=2)),
                              reads=[PR[6]], writes=[r_ostn[ii]])
                        if g == 0 and tt in (3, 15):
                            dump("imp%d" % tt, imp[:, ii].rearrange("p a b -> p (a b)"), [128, 96], F32, [r_imp[ii]])
                            dump("selb%d" % tt, selb[:, ii, :], [128, 32], F32, [r_imp[ii]])
                            dump("oacc%d" % tt, oa[:], [128, 256], F32, [r_oa])
                        row0 = 1024 + g * 256
                        kb.dma("sp", r_ostn[ii], lambda e: e.dma_start(
                            out=omix[row0:row0 + 256, tt * 128:(tt + 1) * 128].rearrange("(a p) t -> p a t", p=128),
                            in_=ostn[ii][:, :, :]), reads=[r_ostn[ii]], writes=[r_omix])

        kb.barrier()
        if debug:
            with contextlib.ExitStack() as ds:
                dt_ = sb(ds, "dbgt", [128, T], BF16)
                r_dt = R("dbgt")
                for k_ in range(16):
                    kb.dma("sp", r_dt, lambda e: e.dma_start(out=dt_[:], in_=omix[k_ * 128:(k_ + 1) * 128, :]),
                           reads=[r_omix], writes=[r_dt])
                    kb.dma("sp", r_dt, lambda e: e.dma_start(out=dbg["omix_o"][k_ * 128:(k_ + 1) * 128, :], in_=dt_[:]),
                           reads=[r_dt], writes=[r_y])

        issue_conv(64)
        kb.barrier()
        with contextlib.ExitStack() as pd:
            wout = sb(pd, "wout", [128, 16, 2048], BF16)
            wq = sb(pd, "wq", [128, 16, 2048], BF16)
            r_wout, r_wq = R("wout"), R("wq")
            for c4 in range(4):
                kb.dma("pool", r_wout, lambda e: e.dma_start(
                    out=wout[:, :, c4 * 512:(c4 + 1) * 512],
                    in_=w_out[0, :, c4 * 512:(c4 + 1) * 512].rearrange("(c p) n -> p c n", p=128)), writes=[r_wout])
            for c4 in range(4):
                kb.dma("pool", r_wq, lambda e: e.dma_start(
                    out=wq[:, :, c4 * 512:(c4 + 1) * 512],
                    in_=peer_w_q[0, :, c4 * 512:(c4 + 1) * 512].rearrange("(c p) n -> p c n", p=128)), writes=[r_wq])
            w2bc = sb(pd, "w2bc", [128, D], F32)
            r_w2bc = R("w2bc")
            kb.dma("sp", r_w2bc, lambda e: e.dma_start(out=w2bc[:], in_=norm2_w[0].partition_broadcast(128)),
                   writes=[r_w2bc])
            KT = sb(pd, "KT", [128, 16, 128], BF16)
            skn = h2T_early = sb(pd, "h2T", [128, 16, 128], BF16)
            r_skn, r_KT = R("skn"), R("KT")
            kb.dma("pool", r_skn, lambda e: e.dma_start(
                out=skn[:], in_=peer_sub_keys[0].rearrange("h two k d -> k (h two) d")), writes=[r_skn])
            for half in range(2):
                bk = half
                pv = PS[bk][:].bitcast(BF16)
                for j in range(8):
                    hp = half * 8 + j
                    kb.op("pe", lambda e: e.transpose(out=pv[:, j * 128:(j + 1) * 128], in_=skn[:, hp, :],
                                                      identity=ident_b[:]),
                          reads=[r_skn, r_const], writes=[PR[bk]])
                kb.op("dve", lambda e: e.tensor_copy(out=KT[:, half * 8:(half + 1) * 8, :],
                                                     in_=pv.rearrange("p (j k) -> p j k", j=8)),
                      reads=[PR[bk]], writes=[r_KT])
            iota16 = sb(pd, "iota16", [128, 16], F32)
            cload(iota16[:], tabs["iota16"][:, :], False)
            _oT = sb(pd, "oT0", [128, 16, 128], BF16)
            oT = [_oT, _oT]
            _roT = R("oT0")
            r_oT = [_roT, _roT]
            _ht = sb(pd, "ht0", [128, D], F32)
            ht = [_ht, _ht]
            _rh = R("ht0")
            r_ht = [_rh, _rh]
            h2 = ht
            r_h2 = r_ht
            h2b = sb(pd, "h2b", [128, D], BF16)
            r_h2b = R("h2b")
            h2T = h2T_early
            r_h2T = r_skn
            junkD = h2b
            r_junkD = r_h2b
            stD = sb(pd, "stD", [128, 16, 4], F32)
            r_stD = [R("stD%d" % i) for i in range(16)]
            qT = sb(pd, "qTp", [128, 16, 128], BF16)
            r_qT = R("qTp")
            scs = [sb(pd, "sc%d" % i, [128, 16, 128], F32) for i in range(2)]
            r_scs = [R("sc0"), R("sc1")]
            pending = None
            tk = sb(pd, "tk", [128, 2, 128], F32)
            vv = sb(pd, "vv", [128, 2, 16], F32)
            iu = sb(pd, "iu", [128, 2, 16], U32)
            iff = sb(pd, "iff", [128, 2, 16], F32)
            cand = sb(pd, "cand", [128, 2, 256], F32)
            cv = sb(pd, "cv", [128, 16], F32)
            pu = sb(pd, "pu", [128, 3, 16], U32)
            pf = sb(pd, "pf", [128, 2, 16], F32)
            oh = sb(pd, "oh", [128, 16, 16], F32)
            isel = sb(pd, "isel", [128, 3, 16], F32)
            gsm = sb(pd, "gsm", [128, 4], F32)
            r_tk = R("tk")

            for tt in range(ND):
                s_ = tt % 2
                sc = scs[s_]
                r_sc = r_scs[s_]
                kb.dma("sp", r_oT[s_], lambda e: e.dma_start(
                    out=oT[s_][:], in_=omix[:, tt * 128:(tt + 1) * 128].rearrange("(k p) t -> p k t", p=128)),
                       reads=[r_omix], writes=[r_oT[s_]])
                kb.dma("sp", r_ht[s_], lambda e: e.dma_start(out=ht[s_][:], in_=x[tt * 128:(tt + 1) * 128, :]),
                       writes=[r_ht[s_]])
                for dmb in range(4):
                    bk = dmb
                    for c in range(16):
                        kb.op("pe", lambda e: e.matmul(PS[bk][:], lhsT=oT[s_][:, c, :],
                                                       rhs=wout[:, c, dmb * 512:(dmb + 1) * 512],
                                                       start=(c == 0), stop=(c == 15)),
                              reads=[r_oT[s_], r_wout], writes=[PR[bk]])
                    kb.op("dve", lambda e: e.tensor_tensor(out=ht[s_][:, dmb * 512:(dmb + 1) * 512], in0=PS[bk][:],
                                                           in1=ht[s_][:, dmb * 512:(dmb + 1) * 512], op=ALU.add),
                          reads=[PR[bk], r_ht[s_]], writes=[r_ht[s_]])
                kb.dma("sp", r_ht[s_], lambda e: e.dma_start(out=hscr[tt * 128:(tt + 1) * 128, :], in_=ht[s_][:]),
                       reads=[r_ht[s_]], writes=[r_hscr])
                if debug:
                    kb.dma("sp", r_ht[s_], lambda e: e.dma_start(out=dbg["h_o"][tt * 128:(tt + 1) * 128, :],
                                                                 in_=ht[s_][:]), reads=[r_ht[s_]], writes=[r_y])
                kb.op("act", lambda e: e.activation(out=junkD[:], in_=ht[s_][:], func=AF.Square),
                      reads=[r_ht[s_]], writes=[r_junkD])
                kb.op("dve", lambda e: e.tensor_reduce(out=stD[:, tt, 0:1], in_=junkD[:], axis=AX.X, op=ALU.add),
                      reads=[r_junkD], writes=[r_stD[tt]])
                kb.op("act", lambda e: e.activation(out=stD[:, tt, 1:2], in_=stD[:, tt, 0:1], func=AF.Sqrt,
                                                    scale=1.0 / D, bias=EPS),
                      reads=[r_stD[tt]], writes=[r_stD[tt]])
                kb.op("dve", lambda e: e.reciprocal(out=stD[:, tt, 2:3], in_=stD[:, tt, 1:2]),
                      reads=[r_stD[tt]], writes=[r_stD[tt]])
                kb.op("dve", lambda e: e.scalar_tensor_tensor(out=h2[s_][:], in0=ht[s_][:], scalar=stD[:, tt, 2:3],
                                                              in1=w2bc[:], op0=ALU.mult, op1=ALU.mult),
                      reads=[r_ht[s_], r_stD[tt], r_w2bc], writes=[r_h2[s_]])
                kb.dma("sp", r_h2[s_], lambda e: e.dma_start(out=h2scr[tt * 128:(tt + 1) * 128, :], in_=h2[s_][:]),
                       reads=[r_h2[s_]], writes=[r_h2scr])
                kb.op("act", lambda e: e.copy(out=h2b[:], in_=h2[s_][:]), reads=[r_h2[s_]], writes=[r_h2b])
                for half in range(2):
                    bk = 4 + half
                    pv = PS[bk][:].bitcast(BF16)
                    for j in range(8):
                        c = half * 8 + j
                        kb.op("pe", lambda e: e.transpose(out=pv[:, j * 128:(j + 1) * 128],
                                                          in_=h2b[:, c * 128:(c + 1) * 128], identity=ident_b[:]),
                              reads=[r_h2b, r_const], writes=[PR[bk]])
                    kb.op("act", lambda e: e.copy(out=h2T[:, half * 8:(half + 1) * 8, :],
                                                  in_=pv.rearrange("p (j t) -> p j t", j=8)),
                          reads=[PR[bk]], writes=[r_h2T])
                for q4 in range(4):
                    bk = 4 + q4 % 2
                    for j in range(4):
                        hp = q4 * 4 + j
                        for c in range(16):
                            kb.op("pe", lambda e: e.matmul(PS[bk][:, j * 128:(j + 1) * 128],
                                                           lhsT=wq[:, c, hp * 128:(hp + 1) * 128], rhs=h2T[:, c, :],
                                                           start=(c == 0), stop=(c == 15)),
                                  reads=[r_wq, r_h2T], writes=[PR[bk]])
                    kb.op("act", lambda e: e.copy(out=qT[:, q4 * 4:(q4 + 1) * 4, :],
                                                  in_=PS[bk][:].rearrange("p (j t) -> p j t", j=4)),
                          reads=[PR[bk]], writes=[r_qT])
                for q4 in range(4):
                    bk = 6 + q4 % 2
                    for j in range(4):
                        hp = q4 * 4 + j
                        kb.op("pe", lambda e: e.matmul(PS[bk][:, j * 128:(j + 1) * 128], lhsT=qT[:, hp, :],
                                                       rhs=KT[:, hp, :], start=True, stop=True),
                              reads=[r_qT, r_KT], writes=[PR[bk]])
                    kb.op("act", lambda e: e.copy(out=sc[:, q4 * 4:(q4 + 1) * 4, :],
                                                  in_=PS[bk][:].rearrange("p (j k) -> p j k", j=4)),
                          reads=[PR[bk]], writes=[r_sc])
                def topk(tt=tt, sc=sc, r_sc=r_sc):
                    for hh in range(8):
                        def dv(fn, extra_r=(), extra_w=()):
                            kb.op("dve", fn, reads=[r_tk, r_sc] + list(extra_r), writes=[r_tk] + list(extra_w))
                        for p2 in range(2):
                            S_ = sc[:, 2 * hh + p2, :]
                            dv(lambda e: e.max(out=vv[:, p2, 0:8], in_=S_))
                            dv(lambda e: e.match_replace(out=tk[:, p2, :], in_to_replace=vv[:, p2, 0:8], in_values=S_,
                                                         imm_value=-3.0e38))
                            dv(lambda e: e.max(out=vv[:, p2, 8:16], in_=tk[:, p2, :]))
                            dv(lambda e: e.max_index(out=iu[:, p2, 0:8], in_max=vv[:, p2, 0:8], in_values=S_))
                            dv(lambda e: e.max_index(out=iu[:, p2, 8:16], in_max=vv[:, p2, 8:16], in_values=S_))
                            dv(lambda e: e.tensor_copy(out=iff[:, p2, :], in_=iu[:, p2, :]))
                        dv(lambda e: e.tensor_tensor(out=cand[:, 0, :].rearrange("p (a b) -> p a b", a=16),
                                                     in0=vv[:, 0, :].unsqueeze(2).to_broadcast([128, 16, 16]),
                                                     in1=vv[:, 1, :].unsqueeze(1).to_broadcast([128, 16, 16]), op=ALU.add))
                        dv(lambda e: e.max(out=cv[:, 0:8], in_=cand[:, 0, :]))
                        dv(lambda e: e.match_replace(out=cand[:, 1, :], in_to_replace=cv[:, 0:8], in_values=cand[:, 0, :],
                                                     imm_value=-3.0e38))
                        dv(lambda e: e.max(out=cv[:, 8:16], in_=cand[:, 1, :]))
                        dv(lambda e: e.max_index(out=pu[:, 0, 0:8], in_max=cv[:, 0:8], in_values=cand[:, 0, :]))
                        dv(lambda e: e.max_index(out=pu[:, 0, 8:16], in_max=cv[:, 8:16], in_values=cand[:, 0, :]))
                        dv(lambda e: e.tensor_single_scalar(out=pu[:, 1, :], in_=pu[:, 0, :], scalar=4,
                                                            op=ALU.logical_shift_right))
                        dv(lambda e: e.tensor_single_scalar(out=pu[:, 2, :], in_=pu[:, 0, :], scalar=15,
                                                            op=ALU.bitwise_and))
                        dv(lambda e: e.tensor_copy(out=pf[:, :, :], in_=pu[:, 1:3, :]))
                        for p2 in range(2):
                            dv(lambda e: e.tensor_tensor(out=oh[:], in0=pf[:, p2, :].unsqueeze(2).to_broadcast([128, 16, 16]),
                                                         in1=iota16[:].unsqueeze(1).to_broadcast([128, 16, 16]),
                                                         op=ALU.is_equal), extra_r=[r_const])
                            dv(lambda e: e.tensor_tensor(out=oh[:], in0=oh[:],
                                                         in1=iff[:, p2, :].unsqueeze(1).to_broadcast([128, 16, 16]),
                                                         op=ALU.mult))
                            dv(lambda e: e.tensor_reduce(out=isel[:, p2, :], in_=oh[:], axis=AX.X, op=ALU.add))
                        dv(lambda e: e.scalar_tensor_tensor(out=isel[:, 2, :], in0=isel[:, 0, :], scalar=128.0,
                                                            in1=isel[:, 1, :], op0=ALU.mult, op1=ALU.add))
                        dv(lambda e: e.tensor_copy(out=eidx[:, tt, hh * 16:(hh + 1) * 16], in_=isel[:, 2, :]),
                           extra_w=[r_eidx[tt]])
                        dv(lambda e: e.tensor_scalar(out=gsm[:, 0:1], in0=cv[:, 0:1], scalar1=-1.0, scalar2=None,
                                                     op0=ALU.mult))
                        kb.op("act", lambda e: e.activation(out=cv[:], in_=cv[:], func=AF.Exp, bias=gsm[:, 0:1]),
                              reads=[r_tk], writes=[r_tk])
                        dv(lambda e: e.tensor_reduce(out=gsm[:, 1:2], in_=cv[:], axis=AX.X, op=ALU.add))
                        dv(lambda e: e.reciprocal(out=gsm[:, 2:3], in_=gsm[:, 1:2]))
                        dv(lambda e: e.tensor_scalar(out=gate[:, tt, hh * 16:(hh + 1) * 16], in0=cv[:],
                                                     scalar1=gsm[:, 2:3], scalar2=None, op0=ALU.mult),
                           extra_w=[r_gate[tt]])
                if pending is not None:
                    pending()
                pending = topk

            if pending is not None:
                pending()
        if debug:
            kb.dma("sp", r_const, lambda e: e.dma_start(out=dbg["eidx_o"][:, :],
                                                        in_=eidx[:].rearrange("p a b -> p (a b)")),
                   reads=r_eidx, writes=[r_y])
            kb.dma("sp", r_const, lambda e: e.dma_start(out=dbg["gate_o"][:, :],
                                                        in_=gate[:].rearrange("p a b -> p (a b)")),
                   reads=r_gate, writes=[r_y])

        kb.barrier()
        with contextlib.ExitStack() as pe_:
            GP = 4
            NB = 12
            uv = [sb(pe_, "uv%d" % i, [128, 2 * D], BF16) for i in range(NB)]
            r_uv = [R("uv%d" % i) for i in range(NB)]
            vsb = [sb(pe_, "vsb%d" % i, [128, D], BF16) for i in range(2)]
            r_vsb = [R("vsb0"), R("vsb1")]
            hE = [sb(pe_, "hE%d" % i, [128, D], F32) for i in range(2)]
            r_hE = [R("hE0"), R("hE1")]
            h2E = [sb(pe_, "h2E%d" % i, [128, D], F32) for i in range(2)]
            r_h2E = [R("h2E0"), R("h2E1")]
            junkE = sb(pe_, "junkE", [128, D], BF16)
            r_junkE = R("junkE")
            actv = [sb(pe_, "actv%d" % i, [128, 128], F32) for i in range(2)]
            r_actv = [[R("actv%d_%d" % (i, k_)) for k_ in range(128)] for i in range(2)]
            gu = sb(pe_, "gu", [128, 2, GP], F32)
            r_gu = R("gu")
            wgt = [sb(pe_, "wgt%d" % i, [128, 128], F32) for i in range(2)]
            r_wgt = [[R("wgt%d_%d" % (i, k_)) for k_ in range(32)] for i in range(2)]
            yt = [sb(pe_, "yt%d" % i, [128, D], F32) for i in range(2)]
            r_yt = [R("yt0"), R("yt1")]
            steps = [(tt, grp) for tt in range(NT if NE else 0) for grp in range(128 // GP)]
            slot_of = {}
            ucnt = 0
            vcnt = 0
            prev = None
            for step in steps + [None]:
                if step is not None:
                    tt, grp = step
                    su = tt % 2
                    if grp == 0:
                        kb.dma("sp", r_h2E[su], lambda e: e.dma_start(out=h2E[su][:],
                                                                      in_=h2scr[tt * 128:(tt + 1) * 128, :]),
                               reads=[r_h2scr], writes=[r_h2E[su]])
                        kb.dma("sp", r_hE[su], lambda e: e.dma_start(out=hE[su][:],
                                                                     in_=hscr[tt * 128:(tt + 1) * 128, :]),
                               reads=[r_hscr], writes=[r_hE[su]])
                    for j in range(GP):
                        hk = grp * GP + j
                        u_ = ucnt % NB
                        ucnt += 1
                        slot_of[(tt, hk)] = u_
                        kb.dma("pool", r_uv[u_], lambda e: e.indirect_dma_start(
                            out=uv[u_][:], out_offset=None, in_=uvbf[:, :],
                            in_offset=bass.IndirectOffsetOnAxis(ap=eidx[:, tt, hk:hk + 1], axis=0)),
                               reads=[r_eidx[tt], r_ubf], writes=[r_uv[u_]])
                        kb.op("dve", lambda e: e.scalar_tensor_tensor(
                            out=junkE[:], in0=uv[u_][:, 0:D], scalar=1.0, in1=h2E[su][:], op0=ALU.mult, op1=ALU.mult,
                            accum_out=actv[su][:, hk:hk + 1]),
                              reads=[r_uv[u_], r_h2E[su]], writes=[r_actv[su][hk]])
                if prev is not None:
                    ptt, pgrp = prev
                    sv = ptt % 2
                    cs = slice(pgrp * GP, (pgrp + 1) * GP)
                    a_ = actv[sv][:, cs]
                    ras = r_actv[sv][pgrp * GP:(pgrp + 1) * GP]
                    if step is None:
                        for _ in range(3):
                            kb.op("dve", lambda e: e.memset(junkE[:, 0:512], 0.0), writes=[r_junkE])
                    kb.op("dve", lambda e: e.tensor_tensor(out=gu[:, 0, :], in0=a_, in1=a_, op=ALU.mult),
                          reads=ras, writes=[r_gu])
                    kb.op("dve", lambda e: e.tensor_scalar(out=gu[:, 0, :], in0=gu[:, 0, :], scalar1=0.044715,
                                                           scalar2=1.0, op0=ALU.mult, op1=ALU.add),
                          reads=[r_gu], writes=[r_gu])
                    kb.op("dve", lambda e: e.tensor_tensor(out=gu[:, 0, :], in0=gu[:, 0, :], in1=a_, op=ALU.mult),
                          reads=[r_gu] + ras, writes=[r_gu])
                    kb.op("act", lambda e: e.activation(out=gu[:, 1, :], in_=gu[:, 0, :], func=AF.Sigmoid,
                                                        scale=1.5957691216),
                          reads=[r_gu], writes=[r_gu])
                    kb.op("dve", lambda e: e.tensor_tensor(out=gu[:, 1, :], in0=gu[:, 1, :], in1=a_, op=ALU.mult),
                          reads=[r_gu] + ras, writes=[r_gu])
                    kb.op("dve", lambda e: e.tensor_tensor(out=wgt[sv][:, cs], in0=gu[:, 1, :], in1=gate[:, ptt, cs],
                                                           op=ALU.mult),
                          reads=[r_gu, r_gate[ptt]], writes=[r_wgt[sv][pgrp]])
                    for j in range(GP):
                        hk = pgrp * GP + j
                        v_ = slot_of.pop((ptt, hk))
                        b_ = vcnt % 2
                        vcnt += 1
                        kb.op("act", lambda e: e.activation(out=vsb[b_][:], in_=uv[v_][:, D:2 * D], func=AF.Copy,
                                                            scale=wgt[sv][:, hk:hk + 1]),
                              reads=[r_uv[v_], r_wgt[sv][pgrp]], writes=[r_vsb[b_]])
                        for dmb in range(4):
                            kb.op("pe", lambda e: e.matmul(PS[dmb][:], lhsT=ident_b[:],
                                                           rhs=vsb[b_][:, dmb * 512:(dmb + 1) * 512],
                                                           start=(hk == 0), stop=(hk == 127)),
                                  reads=[r_vsb[b_], r_const], writes=[PR[dmb]])
                    if pgrp == 128 // GP - 1:
                        for dmb in range(4):
                            kb.op("dve", lambda e: e.tensor_tensor(out=yt[sv][:, dmb * 512:(dmb + 1) * 512],
                                                                   in0=PS[dmb][:],
                                                                   in1=hE[sv][:, dmb * 512:(dmb + 1) * 512], op=ALU.add),
                                  reads=[PR[dmb], r_hE[sv]], writes=[r_yt[sv]])
                        kb.dma("sp", r_yt[sv], lambda e: e.dma_start(out=y[ptt * 128:(ptt + 1) * 128, :],
                                                                     in_=yt[sv][:]),
                               reads=[r_yt[sv]], writes=[r_y])
                prev = step
            kb.wait_all("sp", [r_y] + r_yt)
    nc._dump_list = dump_list
    return nc


_NAMES = ["x", "norm1_w", "w_in", "hg_lb_logits", "hg_norm_w", "q_norm_w", "kc_norm_w", "ks_norm_w", "kw_norm_w",
          "cmp_pos_k", "cmp_pos_v", "w_ck1", "w_ck2", "w_cv1", "w_cv2", "w_out", "norm2_w", "peer_w_q",
          "peer_sub_keys", "peer_u", "peer_v"]


def make_in_maps(inputs):
    tb = host_tables()
    shared = {}
    for k in _NAMES:
        if k == "x":
            continue
        shared[k] = np.ascontiguousarray(np.asarray(inputs[k], dtype=np.float32))
    for k, v in tb.items():
        shared["tb_" + k] = np.ascontiguousarray(v)
    xs = np.asarray(inputs["x"], dtype=np.float32)
    maps = []
    for b in range(8):
        m = dict(shared)
        m["x"] = np.ascontiguousarray(xs[b])
        maps.append(m)
    return maps


def kernel(**inputs):
    nc = build(debug=False)
    in_maps = make_in_maps(inputs)
    res = run_bass_kernel_spmd(nc, in_maps, core_ids=list(range(8)))
    out = np.stack([np.asarray(r["y"], dtype=np.float32) for r in res.results], axis=0)
    return out
```

```python
import contextlib
import numpy as np
import ml_dtypes
import concourse.bass as bass
import concourse.mybir as mybir
from concourse.bass_utils import run_bass_kernel_spmd

F32 = mybir.dt.float32
BF16 = mybir.dt.bfloat16
U32 = mybir.dt.uint32
AF = mybir.ActivationFunctionType
ALU = mybir.AluOpType
AX = mybir.AxisListType

T = 2048
D = 2048
NT = 16
EPS = 1e-6
NEG = -30000.0
C_HQ, C_HF, C_HI, C_HG, C_NQ, C_KC, C_VC, C_KS, C_VS, C_KW, C_VW, C_GT = (
    0, 1024, 2048, 3072, 4096, 5120, 5376, 5632, 5888, 6144, 6400, 6656)
IN_COLS = 6704


class Res:
    def __init__(self, name):
        self.name = name
        self.w = None
        self.r = {}
        self.dsem = None
        self.dcnt = 0


class Eng:
    def __init__(self, name, h, sem):
        self.name = name
        self.h = h
        self.sem = sem
        self.cnt = 0
        self.seen = {}


class KB:
    def __init__(self, nc, stack):
        self.nc = nc
        self.stack = stack
        self.nsem = 0
        self.all_res = []
        self.E = {}
        for name, h in (("pe", nc.tensor), ("dve", nc.vector), ("act", nc.scalar),
                        ("pool", nc.gpsimd), ("sp", nc.sync)):
            self.E[name] = Eng(name, h, self.newsem("e_" + name))

    def newsem(self, name):
        self.nsem += 1
        return self.stack.enter_context(self.nc.semaphore(name))

    def res(self, name):
        r = Res(name)
        self.all_res.append(r)
        return r

    def barrier(self):
        evs = []
        for F in self.E.values():
            if F.cnt > 0:
                evs.append((F.name, F.sem, F.cnt))
        for r in self.all_res:
            if r.dsem is not None and r.dcnt > 0:
                evs.append(("d_" + r.name, r.dsem, r.dcnt))
        for E in self.E.values():
            for ev in evs:
                if ev[0] != E.name:
                    self._wait(E, ev)

    def _wait(self, E, ev):
        key, sem, val = ev
        if E.seen.get(key, 0) < val:
            E.h.wait_ge(sem, val)
            E.seen[key] = val

    def _deps(self, E, reads, writes):
        for r in reads:
            if r.w is not None:
                if not (r.w[0] == E.name and E.name == "pe"):
                    self._wait(E, r.w)
        for w in writes:
            if w.w is not None:
                if not (w.w[0] == E.name and E.name == "pe"):
                    self._wait(E, w.w)
            for ev in w.r.values():
                if ev[0] != E.name:
                    self._wait(E, ev)

    def _mark(self, ev, reads, writes):
        for r in reads:
            r.r[ev[0]] = ev
        for w in writes:
            w.w = ev
            w.r = {}

    def op(self, eng, fn, reads=(), writes=()):
        E = self.E[eng]
        self._deps(E, reads, writes)
        ins = fn(E.h)
        E.cnt += 1
        ins.then_inc(E.sem, 1)
        self._mark((E.name, E.sem, E.cnt), reads, writes)

    def dma(self, eng, q, fn, reads=(), writes=()):
        E = self.E[eng]
        self._deps(E, reads, writes)
        if q.dsem is None:
            q.dsem = self.newsem("d_" + q.name)
        ins = fn(E.h)
        q.dcnt += 16
        ins.then_inc(q.dsem, 16)
        self._mark(("d_" + q.name, q.dsem, q.dcnt), reads, writes)

    def wait_all(self, eng, ress):
        E = self.E[eng]
        for r in ress:
            if r.w is not None:
                self._wait(E, r.w)
            for ev in r.r.values():
                self._wait(E, ev)


def _bf16_split3(v):
    v = np.asarray(v, np.float64)
    hi = v.astype(ml_dtypes.bfloat16).astype(np.float64)
    lo = (v - hi).astype(ml_dtypes.bfloat16).astype(np.float64)
    lo2 = (v - hi - lo).astype(ml_dtypes.bfloat16).astype(np.float64)
    return hi, lo, lo2


def host_tables():
    tb = {}
    tb["ident"] = np.eye(128, dtype=np.float32)
    s = np.arange(128)
    tb["triLE"] = (s[:, None] <= s[None, :]).astype(np.float32)
    tb["triGT"] = (s[:, None] > s[None, :]).astype(np.float32)
    blk = np.zeros((128, 128), np.float32)
    blk[:64, :64] = 1
    blk[64:, 64:] = 1
    tb["onesblk"] = blk
    slopes = 2.0 ** (-8.0 * np.arange(1, 17) / 16)
    slopes = slopes.astype(np.float32).astype(np.float64)
    hi, lo, lo2 = _bf16_split3(slopes)
    sl = np.zeros((9, 16, 128), np.float32)
    for k in range(3):
        sl[3 * k + 0] = hi[:, None]
        sl[3 * k + 1] = lo[:, None]
        sl[3 * k + 2] = lo2[:, None]
    tb["slopeR"] = sl.reshape(9, 16 * 128)
    pl = np.zeros((9, 16, 128), np.float32)
    for dl in range(16):
        pl[0:3, dl, :] = -128.0 * (dl + 1)
        pl[3:6, dl, :] = s[None, :] + 64.0
    tb["posL"] = pl.reshape(9, 16 * 128)
    tb["posLrev"] = pl[:, ::-1, :].copy().reshape(9, 16 * 128)
    pc = np.zeros((9, 16, 128), np.float32)
    for tt in range(16):
        pc[0:3, tt, :] = -128.0 * tt
        pc[3:6, tt, :] = 16.0 * s[None, :]
        pc[6:9, tt, :] = -25.0
    tb["posC"] = pc.reshape(9, 16 * 128)
    n = np.arange(128)
    mc = np.zeros((128, 16, 128), np.float32)
    for tt in range(16):
        mc[:, tt, :] = (16 * n[:, None] + 31 <= 128 * tt + s[None, :])
    mc[127] = 0
    tb["maskC"] = mc.reshape(128, 16 * 128)
    t_pos = np.arange(T)
    cur = t_pos // 64
    j = np.arange(32)
    forced = (j[None, :] == 0) | ((j[None, :] <= cur[:, None]) & (j[None, :] > cur[:, None] - 2))
    future = j[None, :] > cur[:, None]
    m1 = np.where(forced | future, 0.0, 1.0).astype(np.float32)
    m2 = np.where(forced, 1e9, np.where(future, -1e9, 0.0)).astype(np.float32)
    tb["M1"] = m1.reshape(16, 128, 32).transpose(1, 0, 2).reshape(128, 512).copy()
    tb["M2"] = m2.reshape(16, 128, 32).transpose(1, 0, 2).reshape(128, 512).copy()
    E = np.zeros((32, 16, 128), np.float32)
    for st in range(16):
        E[2 * st, st, :64] = 1
        E[2 * st + 1, st, 64:] = 1
    tb["Esel"] = E.reshape(32, 16 * 128)
    cmp_start = np.arange(127) * 16
    sel_start = np.arange(32) * 64
    ov = ((cmp_start[:, None] < sel_start[None, :] + 64)
          & (cmp_start[:, None] + 32 > sel_start[None, :])).astype(np.float32)
    ovp = np.zeros((128, 32), np.float32)
    ovp[:127] = ov
    tb["ovl"] = ovp
    tb["iota16"] = np.tile(np.arange(16, dtype=np.float32)[None, :], (128, 1))
    return tb


TABLE_SHAPES = {k: v.shape for k, v in host_tables().items()}


def build(debug=False, NH=8, NG=4, ND=16, NE=16, dumps=False):
    nc = bass.Bass("TRN2", target_bir_lowering=False)
    di = {}
    dump_list = []

    def din(name, shape, dt=F32):
        di[name] = nc.dram_tensor(name, list(shape), dt, kind="ExternalInput").ap()
        return di[name]

    x = din("x", [T, D])
    norm1_w = din("norm1_w", [1, D])
    w_in = din("w_in", [1, D, IN_COLS])
    hg_lb = din("hg_lb_logits", [2, 1024])
    hg_norm_w = din("hg_norm_w", [1, 128])
    q_norm_w = din("q_norm_w", [1, 64])
    kc_norm_w = din("kc_norm_w", [1, 64])
    ks_norm_w = din("ks_norm_w", [1, 64])
    kw_norm_w = din("kw_norm_w", [1, 64])
    cmp_pos_k = din("cmp_pos_k", [1, 32, 64])
    cmp_pos_v = din("cmp_pos_v", [1, 32, 64])
    w_ck1 = din("w_ck1", [1, 2048, 256])
    w_ck2 = din("w_ck2", [1, 256, 64])
    w_cv1 = din("w_cv1", [1, 2048, 256])
    w_cv2 = din("w_cv2", [1, 256, 64])
    w_out = din("w_out", [1, 2048, 2048])
    norm2_w = din("norm2_w", [1, D])
    peer_w_q = din("peer_w_q", [1, D, 2048])
    peer_sub_keys = din("peer_sub_keys", [1, 8, 2, 128, 128])
    peer_u = din("peer_u", [1, 16384 if NE else 8, D])
    peer_v = din("peer_v", [1, 16384 if NE else 8, D])
    tabs = {k: din("tb_" + k, shp) for k, shp in TABLE_SHAPES.items()}
    y = nc.dram_tensor("y", [T, D], F32, kind="ExternalOutput").ap()
    omix = nc.dram_tensor("omix", [2048, T], BF16, kind="Internal").ap()
    hscr = nc.dram_tensor("hscr", [T, D], F32, kind="Internal").ap()
    h2scr = nc.dram_tensor("h2scr", [T, D], F32, kind="Internal").ap()
    NEXP = 16384 if NE else 8
    uvbf = nc.dram_tensor("uvbf", [NEXP, 2 * D], BF16, kind="Internal").ap()
    dbg = {}
    if debug:
        dbg["omix_o"] = nc.dram_tensor("omix_o", [2048, T], BF16, kind="ExternalOutput").ap()
        dbg["h_o"] = nc.dram_tensor("h_o", [T, D], F32, kind="ExternalOutput").ap()
        dbg["eidx_o"] = nc.dram_tensor("eidx_o", [128, 16 * 128], U32, kind="ExternalOutput").ap()
        dbg["gate_o"] = nc.dram_tensor("gate_o", [128, 16 * 128], F32, kind="ExternalOutput").ap()

    with contextlib.ExitStack() as top:
        kb = KB(nc, top)
        R = kb.res

        def sb(stack, name, shape, dt):
            t = stack.enter_context(nc.sbuf_tensor(name, list(shape), dt))
            return t

        PS = []
        PR = []
        for i in range(8):
            PS.append(top.enter_context(nc.psum_tensor("ps%d" % i, [128, 512], F32)))
            PR.append(R("ps%d" % i))

        ident_f = sb(top, "ident_f", [128, 128], F32)
        ident_b = sb(top, "ident_b", [128, 128], BF16)
        ones_b = sb(top, "ones_b", [128, 128], BF16)
        onesblk_b = sb(top, "onesblk_b", [128, 128], BF16)
        triLE_b = sb(top, "triLE_b", [128, 128], BF16)
        triGT_b = sb(top, "triGT_b", [128, 128], BF16)
        zrow = sb(top, "zrow", [1, 512], BF16)
        r_const = R("const")
        r_constp = R("constp")
        r_omix = R("omix")
        r_hscr = R("hscr")
        r_h2scr = R("h2scr")
        r_y = R("y")
        r_ubf = R("ubf")
        r_vbf = R("vbf")
        conv_jobs = []
        if NE:
            for i in range(32):
                conv_jobs.append((r_ubf, uvbf[:, 0:D], peer_u, i))
                conv_jobs.append((r_ubf, uvbf[:, D:2 * D], peer_v, i))

        def issue_conv(n):
            for _ in range(n):
                if not conv_jobs:
                    return
                rr, dst, src, i = conv_jobs.pop(0)
                kb.dma("pool", rr, lambda e: e.dma_start(out=dst[i * 512:(i + 1) * 512, :],
                                                         in_=src[0, i * 512:(i + 1) * 512, :]), writes=[rr])

        def cload(dst, src, cast):
            if cast:
                kb.dma("pool", r_constp, lambda e: e.dma_start(out=dst, in_=src), writes=[r_constp])
            else:
                kb.dma("sp", r_const, lambda e: e.dma_start(out=dst, in_=src), writes=[r_const])

        def dump(name, ap, shape, dt, reads):
            if not dumps:
                return
            o = nc.dram_tensor("dump_" + name, list(shape), dt, kind="ExternalOutput").ap()
            dump_list.append(name)
            kb.dma("sp", r_const, lambda e: e.dma_start(out=o, in_=ap), reads=list(reads), writes=[r_y])

        cload(ident_f[:], tabs["ident"][:, :], False)
        cload(ident_b[:], tabs["ident"][:, :], True)
        cload(onesblk_b[:], tabs["onesblk"][:, :], True)
        cload(triLE_b[:], tabs["triLE"][:, :], True)
        cload(triGT_b[:], tabs["triGT"][:, :], True)
        kb.op("dve", lambda e: e.memset(ones_b[:], 1.0), writes=[r_const])
        kb.op("dve", lambda e: e.memset(zrow[:], 0.0), writes=[r_const])
        kb.barrier()

        eidx = sb(top, "eidx", [128, 16, 128], U32)
        gate = sb(top, "gate", [128, 16, 128], F32)
        r_eidx = [R("eidx%d" % i) for i in range(16)]
        r_gate = [R("gate%d" % i) for i in range(16)]

        with contextlib.ExitStack() as ms:
            xnT = sb(ms, "xnT", [128, 16, T], BF16)
            r_xnT = R("xnT")
            wslot = [sb(ms, "wslot%d" % i, [128, 16, 512], BF16) for i in range(2)]
            r_wslot = [R("wslot%d" % i) for i in range(2)]
            wcnt = [0]

            def load_wcols(pieces):
                si = wcnt[0] % 2
                wcnt[0] += 1
                off = 0
                for (c0, n) in pieces:
                    src = w_in[0, :, c0:c0 + n].rearrange("(c p) n -> p c n", p=128)
                    dst = wslot[si][:, :, off:off + n]
                    kb.dma("pool", r_wslot[si], lambda e, d=dst, s_=src: e.dma_start(out=d, in_=s_),
                           writes=[r_wslot[si]])
                    off += n
                return wslot[si], r_wslot[si]

            with contextlib.ExitStack() as pa:
                w1bc = sb(pa, "w1bc", [128, D], F32)
                r_w1bc = R("w1bc")
                kb.dma("sp", r_w1bc, lambda e: e.dma_start(out=w1bc[:], in_=norm1_w[0].partition_broadcast(128)),
                       writes=[r_w1bc])
                xt = [sb(pa, "xt%d" % i, [128, D], F32) for i in range(2)]
                r_xt = [R("xt%d" % i) for i in range(2)]
                xnb = [sb(pa, "xnb%d" % i, [128, D], BF16) for i in range(2)]
                r_xnb = [R("xnb%d" % i) for i in range(2)]
                junk = sb(pa, "junkA", [128, D], F32)
                r_junk = R("junkA")
                st = sb(pa, "statA", [128, 16, 4], F32)
                r_st = [R("statA%d" % i) for i in range(16)]
                for tt in range(NT):
                    s_ = tt % 2
                    kb.dma("sp", r_xt[s_], lambda e: e.dma_start(out=xt[s_][:], in_=x[tt * 128:(tt + 1) * 128, :]),
                           writes=[r_xt[s_]])
                    kb.op("act", lambda e: e.activation(out=junk[:], in_=xt[s_][:], func=AF.Square),
                          reads=[r_xt[s_]], writes=[r_junk])
                    kb.op("dve", lambda e: e.tensor_reduce(out=st[:, tt, 0:1], in_=junk[:], axis=AX.X, op=ALU.add),
                          reads=[r_junk], writes=[r_st[tt]])
                    kb.op("act", lambda e: e.activation(out=st[:, tt, 1:2], in_=st[:, tt, 0:1], func=AF.Sqrt,
                                                        scale=1.0 / D, bias=EPS),
                          reads=[r_st[tt]], writes=[r_st[tt]])
                    kb.op("dve", lambda e: e.reciprocal(out=st[:, tt, 2:3], in_=st[:, tt, 1:2]),
                          reads=[r_st[tt]], writes=[r_st[tt]])
                    kb.op("dve", lambda e: e.scalar_tensor_tensor(out=xnb[s_][:], in0=xt[s_][:], scalar=st[:, tt, 2:3],
                                                                  in1=w1bc[:], op0=ALU.mult, op1=ALU.mult),
                          reads=[r_xt[s_], r_st[tt], r_w1bc], writes=[r_xnb[s_]])
                    for half in range(2):
                        bk = half
                        pv = PS[bk][:].bitcast(BF16)
                        for j in range(8):
                            c = half * 8 + j
                            kb.op("pe", lambda e: e.transpose(out=pv[:, j * 128:(j + 1) * 128],
                                                              in_=xnb[s_][:, c * 128:(c + 1) * 128],
                                                              identity=ident_b[:]),
                                  reads=[r_xnb[s_], r_const], writes=[PR[bk]])
                        eng = "act" if half == 0 else "dve"
                        dst = xnT[:, half * 8:(half + 1) * 8, tt * 128:(tt + 1) * 128]
                        src = pv.rearrange("p (j t) -> p j t", j=8)
                        if eng == "act":
                            kb.op("act", lambda e: e.copy(out=dst, in_=src), reads=[PR[bk]], writes=[r_xnT])
                        else:
                            kb.op("dve", lambda e: e.tensor_copy(out=dst, in_=src), reads=[PR[bk]], writes=[r_xnT])

                dump("w1bc", w1bc[:], [128, D], F32, [r_w1bc])
                dump("xt1", xt[1][:], [128, D], F32, [r_xt[1]])
                dump("xnb1", xnb[1][:], [128, D], BF16, [r_xnb[1]])
                dump("identb", ident_b[:], [128, 128], BF16, [r_const])
                dump("stA", st[:].rearrange("p a b -> p (a b)"), [128, 64], F32, r_st)
            kb.barrier()
            with contextlib.ExitStack() as pb:
                lbt = sb(pb, "lbt", [128, 8, 4], F32)
                r_lbt = R("lbt")
                for two in range(2):
                    kb.dma("sp", r_lbt, lambda e: e.dma_start(
                        out=lbt[:, :, two],
                        in_=hg_lb[two].rearrange("(h p) -> p h", p=128),
                        allow_slow_non_contiguous=True), writes=[r_lbt])
                kb.op("dve", lambda e: e.tensor_tensor(out=lbt[:, :, 2:3], in0=lbt[:, :, 0:1], in1=lbt[:, :, 1:2],
                                                       op=ALU.subtract), reads=[r_lbt], writes=[r_lbt])
                kb.op("act", lambda e: e.activation(out=lbt[:, :, 2:3], in_=lbt[:, :, 2:3], func=AF.Sigmoid),
                      reads=[r_lbt], writes=[r_lbt])
                kb.op("dve", lambda e: e.tensor_scalar(out=lbt[:, :, 3:4], in0=lbt[:, :, 2:3], scalar1=-1.0,
                                                       scalar2=1.0, op0=ALU.mult, op1=ALU.add),
                      reads=[r_lbt], writes=[r_lbt])
                hgw = sb(pb, "hgw", [128, 1], F32)
                kb.dma("sp", r_lbt, lambda e: e.dma_start(out=hgw[:], in_=hg_norm_w.rearrange("o p -> p o"),
                                                           allow_slow_non_contiguous=True), writes=[r_lbt])
                rmask = sb(pb, "rmask", [128, T], F32)
                r_rmask = R("rmask")
                kb.op("pool", lambda e: e.memset(rmask[:], 1.0), writes=[r_rmask])
                kb.op("pool", lambda e: e.memset(rmask[:].rearrange("p (n c) -> p n c", c=128)[:, :, 0:1], 0.0),
                      writes=[r_rmask])
                bufA = sb(pb, "hA", [128, T], F32)
                bufB = sb(pb, "hB", [128, T], F32)
                bufC = sb(pb, "hC", [128, T], F32)
                bufD = sb(pb, "hD", [128, T], F32)
                rA, rB, rC, rD = R("hA"), R("hB"), R("hC"), R("hD")
                qd = sb(pb, "qd", [128, T], BF16)
                kd = sb(pb, "kd", [128, T], BF16)
                sg = sb(pb, "sg", [128, T], BF16)
                r_qd, r_kd, r_sg = R("qd"), R("kd"), R("sg")
                vtok = sb(pb, "vtok", [128, 16, 128], BF16)
                kdtok = sb(pb, "kdtok", [128, 16, 128], BF16)
                r_vtok, r_kdtok = R("vtok"), R("kdtok")
                dec = sb(pb, "dec", [128, 16], F32)
                r_dec = R("dec")
                S32 = sb(pb, "S32", [128, 128], F32)
                Sbf = sb(pb, "Sbf", [128, 128], BF16)
                Stmp = sb(pb, "Stmp", [128, 128], F32)
                r_S32, r_Sbf, r_Stmp = R("S32"), R("Sbf"), R("Stmp")
                at = [sb(pb, "at%d" % i, [128, 128], BF16) for i in range(2)]
                r_at = [R("at%d" % i) for i in range(2)]
                sq = [sb(pb, "sq%d" % i, [128, 128], BF16) for i in range(2)]
                r_sq = [R("sq%d" % i) for i in range(2)]
                lnv = [sb(pb, "lnv%d" % i, [128, 128], F32) for i in range(2)]
                r_lnv = [R("lnv%d" % i) for i in range(2)]
                t1 = [sb(pb, "t1%d" % i, [128, 128], F32) for i in range(2)]
                r_t1 = [R("t1%d" % i) for i in range(2)]
                ost = [sb(pb, "ost%d" % i, [128, T], BF16) for i in range(2)]
                r_ost = [R("ost%d" % i) for i in range(2)]

                wl_next = load_wcols([(C_HQ, 128), (C_HF, 128), (C_HI, 128), (C_HG, 128)])
                for h in range(NH):
                    issue_conv(8)
                    wl, r_wl = wl_next
                    if h < NH - 1:
                        o_ = (h + 1) * 128
                        wl_next = load_wcols([(C_HQ + o_, 128), (C_HF + o_, 128), (C_HI + o_, 128), (C_HG + o_, 128)])
                    bkc = 0
                    for qi, kind in ((0, "q"), (1, "f"), (3, "g")):
                        for tb_ in range(4):
                            bk = bkc % 4
                            bkc += 1
                            for c in range(16):
                                kb.op("pe", lambda e: e.matmul(PS[bk][:], lhsT=wl[:, c, qi * 128:(qi + 1) * 128],
                                                               rhs=xnT[:, c, tb_ * 512:(tb_ + 1) * 512],
                                                               start=(c == 0), stop=(c == 15)),
                                      reads=[r_wl, r_xnT], writes=[PR[bk]])
                            sl_ = slice(tb_ * 512, (tb_ + 1) * 512)
                            if kind == "q":
                                kb.op("dve", lambda e: e.tensor_copy(out=bufA[:, sl_], in_=PS[bk][:]),
                                      reads=[PR[bk]], writes=[rA])
                            elif kind == "f":
                                kb.op("act", lambda e: e.activation(out=bufB[:, sl_], in_=PS[bk][:], func=AF.Sigmoid),
                                      reads=[PR[bk]], writes=[rB])
                            else:
                                kb.op("act", lambda e: e.activation(out=sg[:, sl_], in_=PS[bk][:], func=AF.Silu),
                                      reads=[PR[bk]], writes=[r_sg])
                    for grp in range(4):
                        bk = 4
                        for j in range(4):
                            tt = grp * 4 + j
                            for c in range(16):
                                kb.op("pe", lambda e: e.matmul(PS[bk][:, j * 128:(j + 1) * 128],
                                                               lhsT=xnT[:, c, tt * 128:(tt + 1) * 128],
                                                               rhs=wl[:, c, 256:384],
                                                               start=(c == 0), stop=(c == 15)),
                                      reads=[r_wl, r_xnT], writes=[PR[bk]])
                        kb.op("dve", lambda e: e.tensor_copy(out=vtok[:, grp * 4:(grp + 1) * 4, :],
                                                             in_=PS[bk][:].rearrange("p (j v) -> p j v", j=4)),
                              reads=[PR[bk]], writes=[r_vtok])
                    kb.op("dve", lambda e: e.tensor_scalar(out=bufB[:], in0=bufB[:], scalar1=lbt[:, h, 3:4],
                                                           scalar2=lbt[:, h, 2:3], op0=ALU.mult, op1=ALU.add),
                          reads=[rB, r_lbt], writes=[rB])
                    kb.op("act", lambda e: e.activation(out=bufC[:], in_=bufB[:], func=AF.Ln),
                          reads=[rB], writes=[rC])
                    kb.op("dve", lambda e: e.tensor_tensor_scan(out=bufD[:], data0=rmask[:], data1=bufC[:],
                                                                initial=0.0, op0=ALU.mult, op1=ALU.add),
                          reads=[rC, r_rmask], writes=[rD])
                    kb.op("act", lambda e: e.activation(out=bufC[:], in_=bufD[:], func=AF.Exp),
                          reads=[rD], writes=[rC])
                    kb.op("act", lambda e: e.activation(out=bufD[:], in_=bufD[:], func=AF.Exp, scale=-1.0),
                          reads=[rD], writes=[rD])
                    kb.op("dve", lambda e: e.tensor_tensor(out=qd[:], in0=bufA[:], in1=bufC[:], op=ALU.mult),
                          reads=[rA, rC], writes=[r_qd])
                    kb.op("pool", lambda e: e.tensor_copy(
                        out=dec[:], in_=bufC[:].rearrange("p (n c) -> p n c", c=128)[:, :, 127]),
                          reads=[rC], writes=[r_dec])
                    kb.op("dve", lambda e: e.tensor_scalar(out=bufB[:], in0=bufB[:], scalar1=-1.0, scalar2=1.0,
                                                           op0=ALU.mult, op1=ALU.add),
                          reads=[rB], writes=[rB])
                    kb.op("dve", lambda e: e.tensor_tensor(out=kd[:], in0=bufB[:], in1=bufD[:], op=ALU.mult),
                          reads=[rB, rD], writes=[r_kd])
                    for half in range(2):
                        bk = 5
                        pv = PS[bk][:].bitcast(BF16)
                        for j in range(8):
                            n = half * 8 + j
                            kb.op("pe", lambda e: e.transpose(out=pv[:, j * 128:(j + 1) * 128],
                                                              in_=kd[:, n * 128:(n + 1) * 128], identity=ident_b[:]),
                                  reads=[r_kd, r_const], writes=[PR[bk]])
                        kb.op("dve", lambda e: e.tensor_copy(out=kdtok[:, half * 8:(half + 1) * 8, :],
                                                             in_=pv.rearrange("p (j c) -> p j c", j=8)),
                              reads=[PR[bk]], writes=[r_kdtok])
                    if h == 0:
                        dump("xnT0", xnT[:, 0, :], [128, T], BF16, [r_xnT])
                        dump("xnT15", xnT[:, 15, :], [128, T], BF16, [r_xnT])
                        dump("wl", wl[:, 0, :], [128, 512], BF16, [r_wl])
                        dump("q", bufA[:], [128, T], F32, [rA])
                        dump("k", bufB[:], [128, T], F32, [rB])
                        dump("eb", bufC[:], [128, T], F32, [rC])
                        dump("emb", bufD[:], [128, T], F32, [rD])
                        dump("qd", qd[:], [128, T], BF16, [r_qd])
                        dump("kd", kd[:], [128, T], BF16, [r_kd])
                        dump("sg", sg[:], [128, T], BF16, [r_sg])
                        dump("vtok", vtok[:].rearrange("p a b -> p (a b)"), [128, T], BF16, [r_vtok])
                        dump("kdtok", kdtok[:].rearrange("p a b -> p (a b)"), [128, T], BF16, [r_kdtok])
                        dump("dec", dec[:], [128, 16], F32, [r_dec])
                        dump("lbt", lbt[:].rearrange("p a b -> p (a b)"), [128, 32], F32, [r_lbt])
                    os_ = h % 2
                    for n in range(16):
                        a_ = n % 2
                        tsl = slice(n * 128, (n + 1) * 128)
                        pA = PS[6][:, 0:128]
                        kb.op("pe", lambda e: e.matmul(pA, lhsT=kd[:, tsl], rhs=qd[:, tsl], start=True, stop=True),
                              reads=[r_kd, r_qd], writes=[PR[6]])
                        kb.op("dve", lambda e: e.tensor_tensor(out=at[a_][:], in0=pA, in1=triLE_b[:], op=ALU.mult),
                              reads=[PR[6], r_const], writes=[r_at[a_]])
                        pO = PS[7][:, 0:128]
                        kb.op("pe", lambda e: e.matmul(pO, lhsT=vtok[:, n, :], rhs=at[a_][:], start=True,
                                                       stop=(n == 0)),
                              reads=[r_vtok, r_at[a_]], writes=[PR[7]])
                        if n > 0:
                            kb.op("pe", lambda e: e.matmul(pO, lhsT=Sbf[:], rhs=qd[:, tsl], start=False, stop=True),
                                  reads=[r_Sbf, r_qd], writes=[PR[7]])
                        if n < 15:
                            pU = PS[4][:, 0:128]
                            kb.op("pe", lambda e: e.matmul(pU, lhsT=kdtok[:, n, :], rhs=vtok[:, n, :],
                                                           start=True, stop=True),
                                  reads=[r_kdtok, r_vtok], writes=[PR[4]])
                            if n == 0:
                                kb.op("dve", lambda e: e.tensor_copy(out=Stmp[:], in_=pU),
                                      reads=[PR[4]], writes=[r_Stmp])
                            else:
                                kb.op("dve", lambda e: e.tensor_tensor(out=Stmp[:], in0=pU, in1=S32[:], op=ALU.add),
                                      reads=[PR[4], r_S32], writes=[r_Stmp])
                            kb.op("dve", lambda e: e.tensor_scalar(out=S32[:], in0=Stmp[:], scalar1=dec[:, n:n + 1],
                                                                   scalar2=None, op0=ALU.mult),
                                  reads=[r_Stmp, r_dec], writes=[r_S32])
                            kb.op("act", lambda e: e.activation(out=Sbf[:], in_=Stmp[:], func=AF.Copy,
                                                                scale=dec[:, n:n + 1]),
                                  reads=[r_Stmp, r_dec], writes=[r_Sbf])
                        kb.op("act", lambda e: e.activation(out=sq[a_][:], in_=pO, func=AF.Square),
                              reads=[PR[7]], writes=[r_sq[a_]])
                        pS = PS[5][:, 0:128]
                        kb.op("pe", lambda e: e.matmul(pS, lhsT=ones_b[:], rhs=sq[a_][:], start=True, stop=True),
                              reads=[r_sq[a_], r_const], writes=[PR[5]])
                        kb.op("act", lambda e: e.activation(out=lnv[a_][:], in_=pS, func=AF.Ln, scale=1.0 / 128,
                                                            bias=EPS),
                              reads=[PR[5]], writes=[r_lnv[a_]])
                        kb.op("act", lambda e: e.activation(out=lnv[a_][:], in_=lnv[a_][:], func=AF.Exp, scale=-0.5),
                              reads=[r_lnv[a_]], writes=[r_lnv[a_]])
                        kb.op("dve", lambda e: e.tensor_tensor(out=t1[a_][:], in0=pO, in1=lnv[a_][:], op=ALU.mult),
                              reads=[PR[7], r_lnv[a_]], writes=[r_t1[a_]])
                        kb.op("dve", lambda e: e.scalar_tensor_tensor(out=ost[os_][:, tsl], in0=t1[a_][:],
                                                                      scalar=hgw[:, 0:1], in1=sg[:, tsl],
                                                                      op0=ALU.mult, op1=ALU.mult),
                              reads=[r_t1[a_], r_sg, r_lbt], writes=[r_ost[os_]])
                    kb.dma("sp", r_ost[os_], lambda e: e.dma_start(out=omix[h * 128:(h + 1) * 128, :], in_=ost[os_][:]),
                           reads=[r_ost[os_]], writes=[r_omix])
                    if h == 0:
                        dump("ost", ost[os_][:], [128, T], BF16, [r_ost[os_]])
                        dump("S32", S32[:], [128, 128], F32, [r_S32])
                        dump("t1", t1[1][:], [128, 128], F32, [r_t1[1]])
                        dump("lnv", lnv[1][:], [128, 128], F32, [r_lnv[1]])
                        dump("at", at[1][:], [128, 128], BF16, [r_at[1]])

            kb.barrier()
            with contextlib.ExitStack() as pc_:
                slopeR = sb(pc_, "slopeR", [9, 16 * 128], BF16)
                posL = sb(pc_, "posL", [9, 16 * 128], BF16)
                posC = sb(pc_, "posC", [9, 16 * 128], BF16)
                maskC = sb(pc_, "maskC", [128, 16 * 128], BF16)
                Esel = sb(pc_, "Esel", [32, 16 * 128], BF16)
                M1 = sb(pc_, "M1", [128, 512], F32)
                M2 = sb(pc_, "M2", [128, 512], F32)
                cload(slopeR[:], tabs["slopeR"][:, :], True)
                cload(posL[:], tabs["posL"][:, :], True)
                cload(posC[:], tabs["posC"][:, :], True)
                cload(maskC[:], tabs["maskC"][:, :], True)
                cload(Esel[:], tabs["Esel"][:, :], True)
                cload(M1[:], tabs["M1"][:, :], False)
                cload(M2[:], tabs["M2"][:, :], False)
                nw = sb(pc_, "nw", [128, 4], F32)
                kb.op("dve", lambda e: e.memset(nw[:], 0.0), writes=[r_const])
                for (col, src, reps) in ((0, q_norm_w, 2), (1, kc_norm_w, 1), (2, ks_norm_w, 1), (3, kw_norm_w, 1)):
                    for rp in range(reps):
                        kb.dma("sp", r_const, lambda e: e.dma_start(out=nw[rp * 64:(rp + 1) * 64, col:col + 1],
                                                                    in_=src.rearrange("o p -> p o"),
                                                                    allow_slow_non_contiguous=True),
                               writes=[r_const])
                kcn = [sb(pc_, "kcn%d" % i, [128, 4, 128], BF16) for i in range(2)]
                r_kcn = R("kcn")
                vcmp = sb(pc_, "vcmp", [128, 4, 98], BF16)
                r_vcmp = R("vcmp")
                gts = sb(pc_, "gts", [128, 16, 48], F32)
                r_gts = R("gts")
                kb.op("pool", lambda e: e.memset(kcn[1][:], 0.0), writes=[r_kcn])
                kb.op("pool", lambda e: e.memset(vcmp[:], 1.0), writes=[r_vcmp])
                for g in range(4):
                    kb.dma("pool", r_vcmp, lambda e: e.dma_start(out=vcmp[:, g, 65:97], in_=tabs["ovl"][:, :]),
                           writes=[r_vcmp])

                wl, r_wl = load_wcols([(C_GT, 48)])
                for tt in range(NT):
                    bk = tt % 2
                    for c in range(16):
                        kb.op("pe", lambda e: e.matmul(PS[bk][:, 0:48], lhsT=xnT[:, c, tt * 128:(tt + 1) * 128],
                                                       rhs=wl[:, c, 0:48], start=(c == 0), stop=(c == 15)),
                              reads=[r_wl, r_xnT], writes=[PR[bk]])
                    kb.op("act", lambda e: e.activation(out=gts[:, tt, :], in_=PS[bk][:, 0:48], func=AF.Sigmoid),
                          reads=[PR[bk]], writes=[r_gts])

                with contextlib.ExitStack() as pc1:
                    kvc = sb(pc1, "kvc", [128, 4, T], BF16)
                    r_kvc = R("kvc")
                    w1 = sb(pc1, "w1c", [128, 32, 256], BF16)
                    w2 = sb(pc1, "w2c", [128, 2, 2, 128], BF16)
                    posT = sb(pc1, "posT", [128, 32], BF16)
                    r_cw = R("cw")
                    kb.op("pool", lambda e: e.memset(w2[:], 0.0), writes=[r_cw])
                    for wh, (wa, wb, pp) in enumerate(((w_ck1, w_ck2, cmp_pos_k), (w_cv1, w_cv2, cmp_pos_v))):
                        kb.dma("pool", r_cw, lambda e: e.dma_start(
                            out=w1[wh * 64:(wh + 1) * 64, :, :],
                            in_=wa[0].rearrange("(l d) j -> d l j", d=64)), writes=[r_cw])
                        kb.dma("pool", r_cw, lambda e: e.dma_start(
                            out=w2[:, wh, :, 0:64],
                            in_=wb[0].rearrange("(jb j) d -> j jb d", j=128)), writes=[r_cw])
                        kb.dma("pool", r_cw, lambda e: e.dma_start(
                            out=posT[wh * 64:(wh + 1) * 64, :],
                            in_=pp[0].rearrange("l d -> d l"), allow_slow_non_contiguous=True), writes=[r_cw])
                    wl, r_wl = load_wcols([(C_KC, 512)])
                    wkv = sb(pc1, "wkv", [128, 16, 128], BF16)
                    r_wkv = R("wkv")
                    for g in range(4):
                        kb.op("dve", lambda e: e.tensor_copy(out=wkv[:, :, 0:64], in_=wl[:, :, g * 64:(g + 1) * 64]),
                              reads=[r_wl], writes=[r_wkv])
                        kb.op("dve", lambda e: e.tensor_copy(out=wkv[:, :, 64:128],
                                                             in_=wl[:, :, 256 + g * 64:256 + (g + 1) * 64]),
                              reads=[r_wl], writes=[r_wkv])
                        for tb_ in range(4):
                            bk = tb_ % 2
                            for c in range(16):
                                kb.op("pe", lambda e: e.matmul(PS[bk][:], lhsT=wkv[:, c, :],
                                                               rhs=xnT[:, c, tb_ * 512:(tb_ + 1) * 512],
                                                               start=(c == 0), stop=(c == 15)),
                                      reads=[r_wkv, r_xnT], writes=[PR[bk]])
                            kb.op("act", lambda e: e.copy(out=kvc[:, g, tb_ * 512:(tb_ + 1) * 512], in_=PS[bk][:]),
                                  reads=[PR[bk]], writes=[r_kvc])
                    cb = sb(pc1, "cb", [128, 2, 2], F32)
                    r_cb = R("cb")
                    for wh in range(2):
                        ps_ = slice(wh * 64, (wh + 1) * 64)
                        for jb in range(2):
                            bk = 2
                            for l in range(32):
                                kb.op("pe", lambda e: e.matmul(PS[bk][:, 0:1], lhsT=w1[ps_, l, jb * 128:(jb + 1) * 128],
                                                               rhs=posT[ps_, l:l + 1], start=(l == 0), stop=(l == 31)),
                                      reads=[r_cw], writes=[PR[bk]])
                            kb.op("dve", lambda e: e.tensor_copy(out=cb[:, wh, jb:jb + 1], in_=PS[bk][:, 0:1]),
                                  reads=[PR[bk]], writes=[r_cb])
                    xh = sb(pc1, "xh", [128, 128], F32)
                    uu = sb(pc1, "uu", [128, 128], F32)
                    hT = sb(pc1, "hT", [128, 2, 128], BF16)
                    r_xh, r_uu, r_hT = R("xh"), R("uu"), R("hT")
                    sqc = sb(pc1, "sqc", [128, 128], BF16)
                    lnc = sb(pc1, "lnc", [128, 128], F32)
                    r_sqc, r_lnc = R("sqc"), R("lnc")
                    for g in range(4):
                        for wh in range(2):
                            ps_ = slice(wh * 64, (wh + 1) * 64)
                            for jb in range(2):
                                bk = 3
                                for l in range(32):
                                    kb.op("pe", lambda e: e.matmul(
                                        PS[bk][:, 0:127], lhsT=w1[ps_, l, jb * 128:(jb + 1) * 128],
                                        rhs=kvc[ps_, g, l:l + 16 * 126 + 1:16], start=(l == 0), stop=(l == 31)),
                                          reads=[r_cw, r_kvc], writes=[PR[bk]])
                                kb.op("act", lambda e: e.activation(out=xh[:, 0:127], in_=PS[bk][:, 0:127],
                                                                    func=AF.Identity, bias=cb[:, wh, jb:jb + 1]),
                                      reads=[PR[bk], r_cb], writes=[r_xh])
                                kb.op("dve", lambda e: e.tensor_tensor(out=uu[:, 0:127], in0=xh[:, 0:127],
                                                                       in1=xh[:, 0:127], op=ALU.mult),
                                      reads=[r_xh], writes=[r_uu])
                                kb.op("dve", lambda e: e.tensor_scalar(out=uu[:, 0:127], in0=uu[:, 0:127],
                                                                       scalar1=0.044715, scalar2=1.0,
                                                                       op0=ALU.mult, op1=ALU.add),
                                      reads=[r_uu], writes=[r_uu])
                                kb.op("dve", lambda e: e.tensor_tensor(out=uu[:, 0:127], in0=uu[:, 0:127],
                                                                       in1=xh[:, 0:127], op=ALU.mult),
                                      reads=[r_uu, r_xh], writes=[r_uu])
                                kb.op("act", lambda e: e.activation(out=uu[:, 0:127], in_=uu[:, 0:127],
                                                                    func=AF.Sigmoid, scale=1.5957691216),
                                      reads=[r_uu], writes=[r_uu])
                                kb.op("dve", lambda e: e.tensor_tensor(out=hT[:, jb, 0:127], in0=uu[:, 0:127],
                                                                       in1=xh[:, 0:127], op=ALU.mult),
                                      reads=[r_uu, r_xh], writes=[r_hT])
                            if wh == 0:
                                bk = 2
                                for jb in range(2):
                                    kb.op("pe", lambda e: e.matmul(PS[bk][:, 0:127], lhsT=w2[:, 0, jb, :],
                                                                   rhs=hT[:, jb, 0:127], start=(jb == 0), stop=(jb == 1)),
                                          reads=[r_cw, r_hT], writes=[PR[bk]])
                                kb.op("act", lambda e: e.activation(out=sqc[:, 0:127], in_=PS[bk][:, 0:127],
                                                                    func=AF.Square),
                                      reads=[PR[bk]], writes=[r_sqc])
                                kb.op("pe", lambda e: e.matmul(PS[bk][:, 128:255], lhsT=ones_b[:], rhs=sqc[:, 0:127],
                                                               start=True, stop=True),
                                      reads=[r_sqc, r_const], writes=[PR[bk]])
                                kb.op("act", lambda e: e.activation(out=lnc[:, 0:127], in_=PS[bk][:, 128:255],
                                                                    func=AF.Ln, scale=1.0 / 64, bias=EPS),
                                      reads=[PR[bk]], writes=[r_lnc])
                                kb.op("act", lambda e: e.activation(out=lnc[:, 0:127], in_=lnc[:, 0:127],
                                                                    func=AF.Exp, scale=-0.5),
                                      reads=[r_lnc], writes=[r_lnc])
                                kb.op("dve", lambda e: e.scalar_tensor_tensor(
                                    out=kcn[0][:, g, 0:127], in0=PS[bk][:, 0:127], scalar=nw[:, 1:2],
                                    in1=lnc[:, 0:127], op0=ALU.mult, op1=ALU.mult),
                                      reads=[PR[bk], r_lnc, r_const], writes=[r_kcn])
                            else:
                                bk = 2
                                for jb in range(2):
                                    kb.op("pe", lambda e: e.matmul(PS[bk][0:127, 0:64], lhsT=hT[:, jb, 0:127],
                                                                   rhs=w2[:, 1, jb, 0:64], start=(jb == 0), stop=(jb == 1)),
                                          reads=[r_cw, r_hT], writes=[PR[bk]])
                                kb.op("dve", lambda e: e.tensor_copy(out=vcmp[0:127, g, 0:64], in_=PS[bk][0:127, 0:64]),
                                      reads=[PR[bk]], writes=[r_vcmp])
                    kb.dma("sp", r_kcn, lambda e: e.dma_start(out=kcn[1][64:128, :, :], in_=kcn[0][0:64, :, :]),
                           reads=[r_kcn], writes=[r_kcn])

                kb.barrier()
                qn = sb(pc_, "qn", [128, 2, T], BF16)
                r_qn = R("qn")
                kS = [sb(pc_, "kS%d" % i, [128, T], BF16) for i in range(2)]
                kW = [sb(pc_, "kW%d" % i, [128, T], BF16) for i in range(2)]
                r_kS, r_kW = R("kS"), R("kW")
                vS = sb(pc_, "vS", [128, 16, 66], BF16)
                vW = sb(pc_, "vW", [128, 16, 66], BF16)
                r_vS, r_vW = R("vS"), R("vW")
                wpad = sb(pc_, "wpad", [128, 16, 128], BF16)
                r_wpad = R("wpad")
                kb.op("pool", lambda e: e.memset(wpad[:], 0.0), writes=[r_wpad])
                sqn = [sb(pc_, "sqn%d" % i, [128, 512], BF16) for i in range(2)]
                r_sqn = [R("sqn%d" % i) for i in range(2)]
                lnn = [sb(pc_, "lnn%d" % i, [128, 512], F32) for i in range(2)]
                r_lnn = [R("lnn%d" % i) for i in range(2)]
                pT = [sb(pc_, "pT%d" % i, [128, 512], BF16) for i in range(3)]
                r_pT = [R("pT%d" % i) for i in range(3)]
                oacc = [sb(pc_, "oacc%d" % i, [128, 256], F32) for i in range(2)]
                r_oacc = [R("oacc%d" % i) for i in range(2)]
                sm = sb(pc_, "sm", [128, 2, 16], F32)
                r_sm = [R("sm0"), R("sm1")]
                imp = sb(pc_, "imp", [128, 2, 3, 32], F32)
                r_imp = [R("imp0"), R("imp1")]
                mx = sb(pc_, "mx", [128, 2, 16], F32)
                selb = sb(pc_, "selb", [128, 2, 32], F32)
                comb_rhs = [sb(pc_, "combR%d" % i, [64, 512], BF16) for i in range(2)]
                comb_lhs = [sb(pc_, "combL%d" % i, [64, 16 * 128], BF16) for i in range(2)]
                selT = [comb_rhs[0][0:32, :], comb_rhs[1][0:32, :]]
                r_selT = [R("selT0"), R("selT1")]
                r_cl = [R("combL0"), R("combL1")]
                for i_ in range(2):
                    kb.dma("pool", r_cl[i_], lambda e: e.dma_start(out=comb_lhs[i_][0:32, :], in_=tabs["Esel"][:, :]),
                           writes=[r_cl[i_]])
                ostn = [sb(pc_, "ostn%d" % i, [128, 2, 128], BF16) for i in range(2)]
                r_ostn = [R("ostn0"), R("ostn1")]
                for g in range(NG):
                    for i_ in range(2):
                        kb.dma("pool", r_selT[i_], lambda e: e.dma_start(
                            out=comb_rhs[i_][32:41, :], in_=tabs["slopeR"][:, g * 512:(g + 1) * 512]),
                               writes=[r_selT[i_]])
                    kb.op("dve", lambda e: e.memset(kS[1][:], 0.0), writes=[r_kS])
                    kb.op("dve", lambda e: e.memset(kW[1][:], 0.0), writes=[r_kW])
                    kb.op("pool", lambda e: e.memset(vS[:], 1.0), writes=[r_vS])
                    kb.op("pool", lambda e: e.memset(vW[:], 1.0), writes=[r_vW])
                    wl, r_wl = load_wcols([(C_NQ + g * 256, 256), (C_KS + g * 64, 64), (C_VS + g * 64, 64),
                                           (C_KW + g * 64, 64), (C_VW + g * 64, 64)])
                    bkc = 0
                    ncnt = 0
                    for which in range(4):
                        if which >= 2:
                            cs = 256 + (which - 2) * 128
                            kb.op("dve", lambda e: e.tensor_copy(out=wpad[:, :, 0:64], in_=wl[:, :, cs:cs + 64]),
                                  reads=[r_wl], writes=[r_wpad])
                        for tb_ in range(4):
                            bk = bkc % 2
                            bkc += 1
                            for c in range(16):
                                if which < 2:
                                    lh = wl[:, c, which * 128:(which + 1) * 128]
                                    rr = [r_wl, r_xnT]
                                else:
                                    lh = wpad[:, c, :]
                                    rr = [r_wpad, r_xnT]
                                kb.op("pe", lambda e: e.matmul(PS[bk][:], lhsT=lh,
                                                               rhs=xnT[:, c, tb_ * 512:(tb_ + 1) * 512],
                                                               start=(c == 0), stop=(c == 15)),
                                      reads=rr, writes=[PR[bk]])
                            n_ = ncnt % 2
                            ncnt += 1
                            kb.op("act", lambda e: e.activation(out=sqn[n_][:], in_=PS[bk][:], func=AF.Square),
                                  reads=[PR[bk]], writes=[r_sqn[n_]])
                            b2 = 2 + bk
                            ones_ = onesblk_b if which < 2 else ones_b
                            kb.op("pe", lambda e: e.matmul(PS[b2][:], lhsT=ones_[:], rhs=sqn[n_][:],
                                                           start=True, stop=True),
                                  reads=[r_sqn[n_], r_const], writes=[PR[b2]])
                            kb.op("act", lambda e: e.activation(out=lnn[n_][:], in_=PS[b2][:], func=AF.Ln,
                                                                scale=1.0 / 64, bias=EPS),
                                  reads=[PR[b2]], writes=[r_lnn[n_]])
                            kb.op("act", lambda e: e.activation(out=lnn[n_][:], in_=lnn[n_][:], func=AF.Exp,
                                                                scale=-0.5,
                                                                bias=(float(np.log(0.125)) if which < 2 else 0.0)),
                                  reads=[r_lnn[n_]], writes=[r_lnn[n_]])
                            sl_ = slice(tb_ * 512, (tb_ + 1) * 512)
                            if which < 2:
                                dst, rd, wc = qn[:, which, sl_], r_qn, 0
                            elif which == 2:
                                dst, rd, wc = kS[0][:, sl_], r_kS, 2
                            else:
                                dst, rd, wc = kW[0][:, sl_], r_kW, 3
                            kb.op("dve", lambda e: e.scalar_tensor_tensor(out=dst, in0=PS[bk][:], scalar=nw[:, wc:wc + 1],
                                                                          in1=lnn[n_][:], op0=ALU.mult, op1=ALU.mult),
                                  reads=[PR[bk], r_lnn[n_], r_const], writes=[rd])
                    kb.dma("sp", r_kS, lambda e: e.dma_start(out=kS[1][64:128, :], in_=kS[0][0:64, :]),
                           reads=[r_kS], writes=[r_kS])
                    kb.dma("sp", r_kW, lambda e: e.dma_start(out=kW[1][64:128, :], in_=kW[0][0:64, :]),
                           reads=[r_kW], writes=[r_kW])
                    for tt in range(NT):
                        bk = 4 + tt % 2
                        for c in range(16):
                            kb.op("pe", lambda e: e.matmul(PS[bk][:, 0:256], lhsT=xnT[:, c, tt * 128:(tt + 1) * 128],
                                                           rhs=wl[:, c, 256:512], start=(c == 0), stop=(c == 15)),
                                  reads=[r_wl, r_xnT], writes=[PR[bk]])
                        kb.op("act", lambda e: e.copy(out=vS[:, tt, 0:64], in_=PS[bk][:, 64:128]),
                              reads=[PR[bk]], writes=[r_vS])
                        kb.op("act", lambda e: e.copy(out=vW[:, tt, 0:64], in_=PS[bk][:, 192:256]),
                              reads=[PR[bk]], writes=[r_vW])

                    if g == 0:
                        dump("qn", qn[:].rearrange("p a t -> p (a t)"), [128, 2 * T], BF16, [r_qn])
                        dump("kS0", kS[0][:], [128, T], BF16, [r_kS])
                        dump("kS1", kS[1][:], [128, T], BF16, [r_kS])
                        dump("kW0", kW[0][:], [128, T], BF16, [r_kW])
                        dump("vS", vS[:].rearrange("p a b -> p (a b)"), [128, 16 * 66], BF16, [r_vS])
                        dump("vW", vW[:].rearrange("p a b -> p (a b)"), [128, 16 * 66], BF16, [r_vW])
                        dump("kcn0", kcn[0][:].rearrange("p a b -> p (a b)"), [128, 512], BF16, [r_kcn])
                        dump("kcn1", kcn[1][:].rearrange("p a b -> p (a b)"), [128, 512], BF16, [r_kcn])
                        dump("vcmp", vcmp[:].rearrange("p a b -> p (a b)"), [128, 4 * 98], BF16, [r_vcmp])
                        dump("gts", gts[:].rearrange("p a b -> p (a b)"), [128, 16 * 48], F32, [r_gts])
                    scnt = [0]
                    ocnt = [0]

                    def bank(tt, st, branch):
                        sbk = scnt[0] % 3
                        scnt[0] += 1
                        psb = PS[sbk]
                        rps = PR[sbk]
                        p_ = pT[sbk]
                        rp_ = r_pT[sbk]
                        if branch == 0:
                            nk = min(127, 8 * tt + 7)
                            lpos = posC[0:9, tt * 128:tt * 128 + nk]
                        else:
                            nk = 128
                            lpos = posL[0:9, (tt - st) * 128:(tt - st + 1) * 128]
                        if branch == 1:
                            si = tt % 2
                            kb.op("pe", lambda e: e.matmul(
                                psb[:, :], lhsT=comb_lhs[si][0:41, st * 128:(st + 1) * 128],
                                rhs=comb_rhs[si][0:41, :], start=True, stop=False),
                                  reads=[r_cl[si], r_selT[si]], writes=[rps])
                        else:
                            kb.op("pe", lambda e: e.matmul(psb[0:nk, :], lhsT=lpos,
                                                           rhs=slopeR[0:9, g * 512:(g + 1) * 512], start=True,
                                                           stop=False),
                                  reads=[r_const], writes=[rps])
                        for r in range(4):
                            if branch == 0:
                                lh = kcn[r % 2][:, g, 0:nk]
                                rk = r_kcn
                            elif branch == 1:
                                lh = kS[r % 2][:, st * 128:(st + 1) * 128]
                                rk = r_kS
                            else:
                                lh = kW[r % 2][:, st * 128:(st + 1) * 128]
                                rk = r_kW
                            kb.op("pe", lambda e: e.matmul(psb[0:nk, r * 128:(r + 1) * 128], lhsT=lh,
                                                           rhs=qn[:, r // 2, tt * 128:(tt + 1) * 128],
                                                           start=False, stop=(r == 3)),
                                  reads=[rk, r_qn], writes=[rps])
                        kb.op("act", lambda e: e.activation(out=p_[0:nk, :], in_=psb[0:nk, :], func=AF.Exp),
                              reads=[rps], writes=[rp_])
                        msk = None
                        if branch == 0:
                            msk = maskC[0:nk, tt * 128:(tt + 1) * 128]
                        elif st == tt:
                            msk = triLE_b[:, :]
                        elif branch == 2 and st == tt - 4:
                            msk = triGT_b[:, :]
                        if msk is not None:
                            kb.op("dve", lambda e: e.tensor_tensor(
                                out=p_[0:nk, :].rearrange("p (r t) -> p r t", r=4),
                                in0=p_[0:nk, :].rearrange("p (r t) -> p r t", r=4),
                                in1=msk.unsqueeze(1).to_broadcast([nk, 4, 128]), op=ALU.mult),
                                  reads=[rp_, r_const], writes=[rp_])
                        return p_, rp_, nk

                    def finalize(pso, rpo, tt, branch, width, oa, r_oa, ii):
                        if dumps and g == 0 and tt == 3:
                            dscr = sb(pc_, "dscr%d" % branch, [128, 4, 98], F32)
                            r_dscr = R("dscr%d" % branch)
                            kb.op("dve", lambda e: e.tensor_copy(out=dscr[:, :, 0:width], in_=pso[:, :, 0:width]),
                                  reads=[rpo], writes=[r_dscr])
                            dump("pso%d" % branch, dscr[:].rearrange("p a b -> p (a b)"), [128, 392], F32, [r_dscr])
                        kb.op("dve", lambda e: e.tensor_scalar(out=sm[:, ii, 0:4], in0=pso[:, :, 64], scalar1=1e-30,
                                                               scalar2=None, op0=ALU.add),
                              reads=[rpo], writes=[r_sm[ii]])
                        kb.op("dve", lambda e: e.reciprocal(out=sm[:, ii, 4:8], in_=sm[:, ii, 0:4]),
                              reads=[r_sm[ii]], writes=[r_sm[ii]])
                        if branch == 0:
                            for r in range(4):
                                if r == 0:
                                    kb.op("dve", lambda e: e.tensor_scalar(out=imp[:, ii, 0, :], in0=pso[:, 0, 65:97],
                                                                           scalar1=sm[:, ii, 4:5], scalar2=None,
                                                                           op0=ALU.mult),
                                          reads=[rpo, r_sm[ii]], writes=[r_imp[ii]])
                                else:
                                    kb.op("dve", lambda e: e.scalar_tensor_tensor(
                                        out=imp[:, ii, 0, :], in0=pso[:, r, 65:97], scalar=sm[:, ii, 4 + r:5 + r],
                                        in1=imp[:, ii, 0, :], op0=ALU.mult, op1=ALU.add),
                                          reads=[rpo, r_sm[ii], r_imp[ii]], writes=[r_imp[ii]])
                        gv = gts[:, tt, g * 12:(g + 1) * 12].rearrange("p (r b) -> p r b", b=3)[:, :, branch]
                        kb.op("dve", lambda e: e.tensor_tensor(out=sm[:, ii, 8:12], in0=sm[:, ii, 4:8], in1=gv,
                                                               op=ALU.mult),
                              reads=[r_sm[ii], r_gts], writes=[r_sm[ii]])
                        for r in range(4):
                            if branch == 0:
                                kb.op("dve", lambda e: e.tensor_scalar(out=oa[:, r * 64:(r + 1) * 64], in0=pso[:, r, 0:64],
                                                                       scalar1=sm[:, ii, 8 + r:9 + r], scalar2=None,
                                                                       op0=ALU.mult),
                                      reads=[rpo, r_sm[ii]], writes=[r_oa])
                            else:
                                kb.op("dve", lambda e: e.scalar_tensor_tensor(
                                    out=oa[:, r * 64:(r + 1) * 64], in0=pso[:, r, 0:64], scalar=sm[:, ii, 8 + r:9 + r],
                                    in1=oa[:, r * 64:(r + 1) * 64], op0=ALU.mult, op1=ALU.add),
                                      reads=[rpo, r_sm[ii], r_oa], writes=[r_oa])

                    for tt in range(NT):
                        ii = tt % 2
                        oa, r_oa = oacc[ii], r_oacc[ii]
                        kb.dma("pool", r_cl[ii], lambda e: e.dma_start(
                            out=comb_lhs[ii][32:41, 0:(tt + 1) * 128],
                            in_=tabs["posLrev"][:, (15 - tt) * 128:16 * 128]), writes=[r_cl[ii]])
                        p_, rp_, nk = bank(tt, 0, 0)
                        obk = 3 + ocnt[0] % 2
                        ocnt[0] += 1
                        pso = PS[obk][:, 0:392].rearrange("p (r w) -> p r w", r=4)
                        for r in range(4):
                            kb.op("pe", lambda e: e.matmul(pso[:, r, 0:97], lhsT=p_[0:nk, r * 128:(r + 1) * 128],
                                                           rhs=vcmp[0:nk, g, 0:97], start=True, stop=True),
                                  reads=[rp_, r_vcmp], writes=[PR[obk]])
                        if g == 0 and tt == 3:
                            dump("pTc", p_[:], [128, 512], BF16, [rp_])
                        finalize(pso, PR[obk], tt, 0, 97, oa, r_oa, ii)
                        if g == 0 and tt == 3:
                            dump("sm0", sm[:].rearrange("p a b -> p (a b)"), [128, 32], F32, r_sm)
                        kb.op("dve", lambda e: e.tensor_tensor(out=imp[:, ii, 1, :], in0=imp[:, ii, 0, :],
                                                               in1=M1[:, tt * 32:(tt + 1) * 32], op=ALU.mult),
                              reads=[r_imp[ii], r_const], writes=[r_imp[ii]])
                        kb.op("dve", lambda e: e.tensor_tensor(out=imp[:, ii, 1, :], in0=imp[:, ii, 1, :],
                                                               in1=M2[:, tt * 32:(tt + 1) * 32], op=ALU.add),
                              reads=[r_imp[ii], r_const], writes=[r_imp[ii]])
                        kb.op("dve", lambda e: e.max(out=mx[:, ii, 0:8], in_=imp[:, ii, 1, :]),
                              reads=[r_imp[ii]], writes=[r_imp[ii]])
                        kb.op("dve", lambda e: e.match_replace(out=imp[:, ii, 2, :], in_to_replace=mx[:, ii, 0:8],
                                                               in_values=imp[:, ii, 1, :], imm_value=-3.0e38),
                              reads=[r_imp[ii]], writes=[r_imp[ii]])
                        kb.op("dve", lambda e: e.max(out=mx[:, ii, 8:16], in_=imp[:, ii, 2, :]),
                              reads=[r_imp[ii]], writes=[r_imp[ii]])
                        kb.op("dve", lambda e: e.tensor_scalar(out=selb[:, ii, :], in0=imp[:, ii, 1, :],
                                                               scalar1=mx[:, ii, 15:16], scalar2=NEG,
                                                               op0=ALU.is_lt, op1=ALU.mult),
                              reads=[r_imp[ii]], writes=[r_imp[ii]])
                        kb.op("pe", lambda e: e.transpose(out=PS[5][0:32, 0:128], in_=selb[:, ii, :],
                                                          identity=ident_f[:]),
                              reads=[r_imp[ii], r_const], writes=[PR[5]])
                        kb.op("act", lambda e: e.copy(
                            out=selT[ii].rearrange("p (r t) -> p r t", r=4),
                            in_=PS[5][0:32, 0:128].unsqueeze(1).to_broadcast([32, 4, 128])),
                              reads=[PR[5]], writes=[r_selT[ii]])
                        for branch in (1, 2):
                            obk = 3 + ocnt[0] % 2
                            ocnt[0] += 1
                            pso = PS[obk][:, 0:264].rearrange("p (r w) -> p r w", r=4)
                            kb.op("pe", lambda e: e.matmul(PS[obk][:, 0:264], lhsT=zrow[0:1, 0:128],
                                                           rhs=zrow[0:1, 0:264], start=True, stop=False),
                                  reads=[r_const], writes=[PR[obk]])
                            st0 = 0 if branch == 1 else max(0, tt - 4)
                            vt, rv = (vS, r_vS) if branch == 1 else (vW, r_vW)
                            for st in range(st0, tt + 1):
                                p_, rp_, nk = bank(tt, st, branch)
                                for r in range(4):
                                    kb.op("pe", lambda e: e.matmul(pso[:, r, 0:65], lhsT=p_[:, r * 128:(r + 1) * 128],
                                                                   rhs=vt[:, st, 0:65], start=False,
                                                                   stop=(st == tt and r == 3)),
                                          reads=[rp_, rv], writes=[PR[obk]])
                            finalize(pso, PR[obk], tt, branch, 66, oa, r_oa, ii)
                        for hf in range(2):
                            kb.op("pe", lambda e: e.transpose(out=PS[6][:, hf * 128:(hf + 1) * 128],
                                                              in_=oa[:, hf * 128:(hf + 1) * 128], identity=ident_f[:]),
                                  reads=[r_oa, r_const], writes=[PR[6]])
                        kb.op("act", lambda e: e.copy(out=ostn[ii][:, :, :],
                                                      in_=PS[6][:, 0:256].rearrange("p (a t) -> p a t", a=2)),
                              reads=[PR[6]], writes=[r_ostn[ii]])
                        if g == 0 and tt in (3, 15):
                            dump("imp%d" % tt, imp[:, ii].rearrange("p a b -> p (a b)"), [128, 96], F32, [r_imp[ii]])
                            dump("selb%d" % tt, selb[:, ii, :], [128, 32], F32, [r_imp[ii]])
                            dump("oacc%d" % tt, oa[:], [128, 256], F32, [r_oa])
                        row0 = 1024 + g * 256
                        kb.dma("sp", r_ostn[ii], lambda e: e.dma_start(
                            out=omix[row0:row0 + 256, tt * 128:(tt + 1) * 128].rearrange("(a p) t -> p a t", p=128),
                            in_=ostn[ii][:, :, :]), reads=[r_ostn[ii]], writes=[r_omix])

        kb.barrier()
        if debug:
            with contextlib.ExitStack() as ds:
                dt_ = sb(ds, "dbgt", [128, T], BF16)
                r_dt = R("dbgt")
                for k_ in range(16):
                    kb.dma("sp", r_dt, lambda e: e.dma_start(out=dt_[:], in_=omix[k_ * 128:(k_ + 1) * 128, :]),
                           reads=[r_omix], writes=[r_dt])
                    kb.dma("sp", r_dt, lambda e: e.dma_start(out=dbg["omix_o"][k_ * 128:(k_ + 1) * 128, :], in_=dt_[:]),
                           reads=[r_dt], writes=[r_y])

        issue_conv(64)
        kb.barrier()
        with contextlib.ExitStack() as pd:
            wout = sb(pd, "wout", [128, 16, 2048], BF16)
            wq = sb(pd, "wq", [128, 16, 2048], BF16)
            r_wout, r_wq = R("wout"), R("wq")
            for c4 in range(4):
                kb.dma("pool", r_wout, lambda e: e.dma_start(
                    out=wout[:, :, c4 * 512:(c4 + 1) * 512],
                    in_=w_out[0, :, c4 * 512:(c4 + 1) * 512].rearrange("(c p) n -> p c n", p=128)), writes=[r_wout])
            for c4 in range(4):
                kb.dma("pool", r_wq, lambda e: e.dma_start(
                    out=wq[:, :, c4 * 512:(c4 + 1) * 512],
                    in_=peer_w_q[0, :, c4 * 512:(c4 + 1) * 512].rearrange("(c p) n -> p c n", p=128)), writes=[r_wq])
            w2bc = sb(pd, "w2bc", [128, D], F32)
            r_w2bc = R("w2bc")
            kb.dma("sp", r_w2bc, lambda e: e.dma_start(out=w2bc[:], in_=norm2_w[0].partition_broadcast(128)),
                   writes=[r_w2bc])
            KT = sb(pd, "KT", [128, 16, 128], BF16)
            skn = h2T_early = sb(pd, "h2T", [128, 16, 128], BF16)
            r_skn, r_KT = R("skn"), R("KT")
            kb.dma("pool", r_skn, lambda e: e.dma_start(
                out=skn[:], in_=peer_sub_keys[0].rearrange("h two k d -> k (h two) d")), writes=[r_skn])
            for half in range(2):
                bk = half
                pv = PS[bk][:].bitcast(BF16)
                for j in range(8):
                    hp = half * 8 + j
                    kb.op("pe", lambda e: e.transpose(out=pv[:, j * 128:(j + 1) * 128], in_=skn[:, hp, :],
                                                      identity=ident_b[:]),
                          reads=[r_skn, r_const], writes=[PR[bk]])
                kb.op("dve", lambda e: e.tensor_copy(out=KT[:, half * 8:(half + 1) * 8, :],
                                                     in_=pv.rearrange("p (j k) -> p j k", j=8)),
                      reads=[PR[bk]], writes=[r_KT])
            iota16 = sb(pd, "iota16", [128, 16], F32)
            cload(iota16[:], tabs["iota16"][:, :], False)
            _oT = sb(pd, "oT0", [128, 16, 128], BF16)
            oT = [_oT, _oT]
            _roT = R("oT0")
            r_oT = [_roT, _roT]
            _ht = sb(pd, "ht0", [128, D], F32)
            ht = [_ht, _ht]
            _rh = R("ht0")
            r_ht = [_rh, _rh]
            h2 = ht
            r_h2 = r_ht
            h2b = sb(pd, "h2b", [128, D], BF16)
            r_h2b = R("h2b")
            h2T = h2T_early
            r_h2T = r_skn
            junkD = h2b
            r_junkD = r_h2b
            stD = sb(pd, "stD", [128, 16, 4], F32)
            r_stD = [R("stD%d" % i) for i in range(16)]
            qT = sb(pd, "qTp", [128, 16, 128], BF16)
            r_qT = R("qTp")
            scs = [sb(pd, "sc%d" % i, [128, 16, 128], F32) for i in range(2)]
            r_scs = [R("sc0"), R("sc1")]
            pending = None
            tk = sb(pd, "tk", [128, 2, 128], F32)
            vv = sb(pd, "vv", [128, 2, 16], F32)
            iu = sb(pd, "iu", [128, 2, 16], U32)
            iff = sb(pd, "iff", [128, 2, 16], F32)
            cand = sb(pd, "cand", [128, 2, 256], F32)
            cv = sb(pd, "cv", [128, 16], F32)
            pu = sb(pd, "pu", [128, 3, 16], U32)
            pf = sb(pd, "pf", [128, 2, 16], F32)
            oh = sb(pd, "oh", [128, 16, 16], F32)
            isel = sb(pd, "isel", [128, 3, 16], F32)
            gsm = sb(pd, "gsm", [128, 4], F32)
            r_tk = R("tk")

            for tt in range(ND):
                s_ = tt % 2
                sc = scs[s_]
                r_sc = r_scs[s_]
                kb.dma("sp", r_oT[s_], lambda e: e.dma_start(
                    out=oT[s_][:], in_=omix[:, tt * 128:(tt + 1) * 128].rearrange("(k p) t -> p k t", p=128)),
                       reads=[r_omix], writes=[r_oT[s_]])
                kb.dma("sp", r_ht[s_], lambda e: e.dma_start(out=ht[s_][:], in_=x[tt * 128:(tt + 1) * 128, :]),
                       writes=[r_ht[s_]])
                for dmb in range(4):
                    bk = dmb
                    for c in range(16):
                        kb.op("pe", lambda e: e.matmul(PS[bk][:], lhsT=oT[s_][:, c, :],
                                                       rhs=wout[:, c, dmb * 512:(dmb + 1) * 512],
                                                       start=(c == 0), stop=(c == 15)),
                              reads=[r_oT[s_], r_wout], writes=[PR[bk]])
                    kb.op("dve", lambda e: e.tensor_tensor(out=ht[s_][:, dmb * 512:(dmb + 1) * 512], in0=PS[bk][:],
                                                           in1=ht[s_][:, dmb * 512:(dmb + 1) * 512], op=ALU.add),
                          reads=[PR[bk], r_ht[s_]], writes=[r_ht[s_]])
                kb.dma("sp", r_ht[s_], lambda e: e.dma_start(out=hscr[tt * 128:(tt + 1) * 128, :], in_=ht[s_][:]),
                       reads=[r_ht[s_]], writes=[r_hscr])
                if debug:
                    kb.dma("sp", r_ht[s_], lambda e: e.dma_start(out=dbg["h_o"][tt * 128:(tt + 1) * 128, :],
                                                                 in_=ht[s_][:]), reads=[r_ht[s_]], writes=[r_y])
                kb.op("act", lambda e: e.activation(out=junkD[:], in_=ht[s_][:], func=AF.Square),
                      reads=[r_ht[s_]], writes=[r_junkD])
                kb.op("dve", lambda e: e.tensor_reduce(out=stD[:, tt, 0:1], in_=junkD[:], axis=AX.X, op=ALU.add),
                      reads=[r_junkD], writes=[r_stD[tt]])
                kb.op("act", lambda e: e.activation(out=stD[:, tt, 1:2], in_=stD[:, tt, 0:1], func=AF.Sqrt,
                                                    scale=1.0 / D, bias=EPS),
                      reads=[r_stD[tt]], writes=[r_stD[tt]])
                kb.op("dve", lambda e: e.reciprocal(out=stD[:, tt, 2:3], in_=stD[:, tt, 1:2]),
                      reads=[r_stD[tt]], writes=[r_stD[tt]])
                kb.op("dve", lambda e: e.scalar_tensor_tensor(out=h2[s_][:], in0=ht[s_][:], scalar=stD[:, tt, 2:3],
                                                              in1=w2bc[:], op0=ALU.mult, op1=ALU.mult),
                      reads=[r_ht[s_], r_stD[tt], r_w2bc], writes=[r_h2[s_]])
                kb.dma("sp", r_h2[s_], lambda e: e.dma_start(out=h2scr[tt * 128:(tt + 1) * 128, :], in_=h2[s_][:]),
                       reads=[r_h2[s_]], writes=[r_h2scr])
                kb.op("act", lambda e: e.copy(out=h2b[:], in_=h2[s_][:]), reads=[r_h2[s_]], writes=[r_h2b])
                for half in range(2):
                    bk = 4 + half
                    pv = PS[bk][:].bitcast(BF16)
                    for j in range(8):
                        c = half * 8 + j
                        kb.op("pe", lambda e: e.transpose(out=pv[:, j * 128:(j + 1) * 128],
                                                          in_=h2b[:, c * 128:(c + 1) * 128], identity=ident_b[:]),
                              reads=[r_h2b, r_const], writes=[PR[bk]])
                    kb.op("act", lambda e: e.copy(out=h2T[:, half * 8:(half + 1) * 8, :],
                                                  in_=pv.rearrange("p (j t) -> p j t", j=8)),
                          reads=[PR[bk]], writes=[r_h2T])
                for q4 in range(4):
                    bk = 4 + q4 % 2
                    for j in range(4):
                        hp = q4 * 4 + j
                        for c in range(16):
                            kb.op("pe", lambda e: e.matmul(PS[bk][:, j * 128:(j + 1) * 128],
                                                           lhsT=wq[:, c, hp * 128:(hp + 1) * 128], rhs=h2T[:, c, :],
                                                           start=(c == 0), stop=(c == 15)),
                                  reads=[r_wq, r_h2T], writes=[PR[bk]])
                    kb.op("act", lambda e: e.copy(out=qT[:, q4 * 4:(q4 + 1) * 4, :],
                                                  in_=PS[bk][:].rearrange("p (j t) -> p j t", j=4)),
                          reads=[PR[bk]], writes=[r_qT])
                for q4 in range(4):
                    bk = 6 + q4 % 2
                    for j in range(4):
                        hp = q4 * 4 + j
                        kb.op("pe", lambda e: e.matmul(PS[bk][:, j * 128:(j + 1) * 128], lhsT=qT[:, hp, :],
                                                       rhs=KT[:, hp, :], start=True, stop=True),
                              reads=[r_qT, r_KT], writes=[PR[bk]])
                    kb.op("act", lambda e: e.copy(out=sc[:, q4 * 4:(q4 + 1) * 4, :],
                                                  in_=PS[bk][:].rearrange("p (j k) -> p j k", j=4)),
                          reads=[PR[bk]], writes=[r_sc])
                def topk(tt=tt, sc=sc, r_sc=r_sc):
                    for hh in range(8):
                        def dv(fn, extra_r=(), extra_w=()):
                            kb.op("dve", fn, reads=[r_tk, r_sc] + list(extra_r), writes=[r_tk] + list(extra_w))
                        for p2 in range(2):
                            S_ = sc[:, 2 * hh + p2, :]
                            dv(lambda e: e.max(out=vv[:, p2, 0:8], in_=S_))
                            dv(lambda e: e.match_replace(out=tk[:, p2, :], in_to_replace=vv[:, p2, 0:8], in_values=S_,
                                                         imm_value=-3.0e38))
                            dv(lambda e: e.max(out=vv[:, p2, 8:16], in_=tk[:, p2, :]))
                            dv(lambda e: e.max_index(out=iu[:, p2, 0:8], in_max=vv[:, p2, 0:8], in_values=S_))
                            dv(lambda e: e.max_index(out=iu[:, p2, 8:16], in_max=vv[:, p2, 8:16], in_values=S_))
                            dv(lambda e: e.tensor_copy(out=iff[:, p2, :], in_=iu[:, p2, :]))
                        dv(lambda e: e.tensor_tensor(out=cand[:, 0, :].rearrange("p (a b) -> p a b", a=16),
                                                     in0=vv[:, 0, :].unsqueeze(2).to_broadcast([128, 16, 16]),
                                                     in1=vv[:, 1, :].unsqueeze(1).to_broadcast([128, 16, 16]), op=ALU.add))
                        dv(lambda e: e.max(out=cv[:, 0:8], in_=cand[:, 0, :]))
                        dv(lambda e: e.match_replace(out=cand[:, 1, :], in_to_replace=cv[:, 0:8], in_values=cand[:, 0, :],
                                                     imm_value=-3.0e38))
                        dv(lambda e: e.max(out=cv[:, 8:16], in_=cand[:, 1, :]))
                        dv(lambda e: e.max_index(out=pu[:, 0, 0:8], in_max=cv[:, 0:8], in_values=cand[:, 0, :]))
                        dv(lambda e: e.max_index(out=pu[:, 0, 8:16], in_max=cv[:, 8:16], in_values=cand[:, 0, :]))
                        dv(lambda e: e.tensor_single_scalar(out=pu[:, 1, :], in_=pu[:, 0, :], scalar=4,
                                                            op=ALU.logical_shift_right))
                        dv(lambda e: e.tensor_single_scalar(out=pu[:, 2, :], in_=pu[:, 0, :], scalar=15,
                                                            op=ALU.bitwise_and))
                        dv(lambda e: e.tensor_copy(out=pf[:, :, :], in_=pu[:, 1:3, :]))
                        for p2 in range(2):
                            dv(lambda e: e.tensor_tensor(out=oh[:], in0=pf[:, p2, :].unsqueeze(2).to_broadcast([128, 16, 16]),
                                                         in1=iota16[:].unsqueeze(1).to_broadcast([128, 16, 16]),
                                                         op=ALU.is_equal), extra_r=[r_const])
                            dv(lambda e: e.tensor_tensor(out=oh[:], in0=oh[:],
                                                         in1=iff[:, p2, :].unsqueeze(1).to_broadcast([128, 16, 16]),
                                                         op=ALU.mult))
                            dv(lambda e: e.tensor_reduce(out=isel[:, p2, :], in_=oh[:], axis=AX.X, op=ALU.add))
                        dv(lambda e: e.scalar_tensor_tensor(out=isel[:, 2, :], in0=isel[:, 0, :], scalar=128.0,
                                                            in1=isel[:, 1, :], op0=ALU.mult, op1=ALU.add))
                        dv(lambda e: e.tensor_copy(out=eidx[:, tt, hh * 16:(hh + 1) * 16], in_=isel[:, 2, :]),
                           extra_w=[r_eidx[tt]])
                        dv(lambda e: e.tensor_scalar(out=gsm[:, 0:1], in0=cv[:, 0:1], scalar1=-1.0, scalar2=None,
                                                     op0=ALU.mult))
                        kb.op("act", lambda e: e.activation(out=cv[:], in_=cv[:], func=AF.Exp, bias=gsm[:, 0:1]),
                              reads=[r_tk], writes=[r_tk])
                        dv(lambda e: e.tensor_reduce(out=gsm[:, 1:2], in_=cv[:], axis=AX.X, op=ALU.add))
                        dv(lambda e: e.reciprocal(out=gsm[:, 2:3], in_=gsm[:, 1:2]))
                        dv(lambda e: e.tensor_scalar(out=gate[:, tt, hh * 16:(hh + 1) * 16], in0=cv[:],
                                                     scalar1=gsm[:, 2:3], scalar2=None, op0=ALU.mult),
                           extra_w=[r_gate[tt]])
                if pending is not None:
                    pending()
                pending = topk

            if pending is not None:
                pending()
        if debug:
            kb.dma("sp", r_const, lambda e: e.dma_start(out=dbg["eidx_o"][:, :],
                                                        in_=eidx[:].rearrange("p a b -> p (a b)")),
                   reads=r_eidx, writes=[r_y])
            kb.dma("sp", r_const, lambda e: e.dma_start(out=dbg["gate_o"][:, :],
                                                        in_=gate[:].rearrange("p a b -> p (a b)")),
                   reads=r_gate, writes=[r_y])

        kb.barrier()
        with contextlib.ExitStack() as pe_:
            GP = 4
            NB = 12
            uv = [sb(pe_, "uv%d" % i, [128, 2 * D], BF16) for i in range(NB)]
            r_uv = [R("uv%d" % i) for i in range(NB)]
            vsb = [sb(pe_, "vsb%d" % i, [128, D], BF16) for i in range(2)]
            r_vsb = [R("vsb0"), R("vsb1")]
            hE = [sb(pe_, "hE%d" % i, [128, D], F32) for i in range(2)]
            r_hE = [R("hE0"), R("hE1")]
            h2E = [sb(pe_, "h2E%d" % i, [128, D], F32) for i in range(2)]
            r_h2E = [R("h2E0"), R("h2E1")]
            junkE = sb(pe_, "junkE", [128, D], BF16)
            r_junkE = R("junkE")
            actv = [sb(pe_, "actv%d" % i, [128, 128], F32) for i in range(2)]
            r_actv = [[R("actv%d_%d" % (i, k_)) for k_ in range(128)] for i in range(2)]
            gu = sb(pe_, "gu", [128, 2, GP], F32)
            r_gu = R("gu")
            wgt = [sb(pe_, "wgt%d" % i, [128, 128], F32) for i in range(2)]
            r_wgt = [[R("wgt%d_%d" % (i, k_)) for k_ in range(32)] for i in range(2)]
            yt = [sb(pe_, "yt%d" % i, [128, D], F32) for i in range(2)]
            r_yt = [R("yt0"), R("yt1")]
            steps = [(tt, grp) for tt in range(NT if NE else 0) for grp in range(128 // GP)]
            slot_of = {}
            ucnt = 0
            vcnt = 0
            prev = None
            for step in steps + [None]:
                if step is not None:
                    tt, grp = step
                    su = tt % 2
                    if grp == 0:
                        kb.dma("sp", r_h2E[su], lambda e: e.dma_start(out=h2E[su][:],
                                                                      in_=h2scr[tt * 128:(tt + 1) * 128, :]),
                               reads=[r_h2scr], writes=[r_h2E[su]])
                        kb.dma("sp", r_hE[su], lambda e: e.dma_start(out=hE[su][:],
                                                                     in_=hscr[tt * 128:(tt + 1) * 128, :]),
                               reads=[r_hscr], writes=[r_hE[su]])
                    for j in range(GP):
                        hk = grp * GP + j
                        u_ = ucnt % NB
                        ucnt += 1
                        slot_of[(tt, hk)] = u_
                        kb.dma("pool", r_uv[u_], lambda e: e.indirect_dma_start(
                            out=uv[u_][:], out_offset=None, in_=uvbf[:, :],
                            in_offset=bass.IndirectOffsetOnAxis(ap=eidx[:, tt, hk:hk + 1], axis=0)),
                               reads=[r_eidx[tt], r_ubf], writes=[r_uv[u_]])
                        kb.op("dve", lambda e: e.scalar_tensor_tensor(
                            out=junkE[:], in0=uv[u_][:, 0:D], scalar=1.0, in1=h2E[su][:], op0=ALU.mult, op1=ALU.mult,
                            accum_out=actv[su][:, hk:hk + 1]),
                              reads=[r_uv[u_], r_h2E[su]], writes=[r_actv[su][hk]])
                if prev is not None:
                    ptt, pgrp = prev
                    sv = ptt % 2
                    cs = slice(pgrp * GP, (pgrp + 1) * GP)
                    a_ = actv[sv][:, cs]
                    ras = r_actv[sv][pgrp * GP:(pgrp + 1) * GP]
                    if step is None:
                        for _ in range(3):
                            kb.op("dve", lambda e: e.memset(junkE[:, 0:512], 0.0), writes=[r_junkE])
                    kb.op("dve", lambda e: e.tensor_tensor(out=gu[:, 0, :], in0=a_, in1=a_, op=ALU.mult),
                          reads=ras, writes=[r_gu])
                    kb.op("dve", lambda e: e.tensor_scalar(out=gu[:, 0, :], in0=gu[:, 0, :], scalar1=0.044715,
                                                           scalar2=1.0, op0=ALU.mult, op1=ALU.add),
                          reads=[r_gu], writes=[r_gu])
                    kb.op("dve", lambda e: e.tensor_tensor(out=gu[:, 0, :], in0=gu[:, 0, :], in1=a_, op=ALU.mult),
                          reads=[r_gu] + ras, writes=[r_gu])
                    kb.op("act", lambda e: e.activation(out=gu[:, 1, :], in_=gu[:, 0, :], func=AF.Sigmoid,
                                                        scale=1.5957691216),
                          reads=[r_gu], writes=[r_gu])
                    kb.op("dve", lambda e: e.tensor_tensor(out=gu[:, 1, :], in0=gu[:, 1, :], in1=a_, op=ALU.mult),
                          reads=[r_gu] + ras, writes=[r_gu])
                    kb.op("dve", lambda e: e.tensor_tensor(out=wgt[sv][:, cs], in0=gu[:, 1, :], in1=gate[:, ptt, cs],
                                                           op=ALU.mult),
                          reads=[r_gu, r_gate[ptt]], writes=[r_wgt[sv][pgrp]])
                    for j in range(GP):
                        hk = pgrp * GP + j
                        v_ = slot_of.pop((ptt, hk))
                        b_ = vcnt % 2
                        vcnt += 1
                        kb.op("act", lambda e: e.activation(out=vsb[b_][:], in_=uv[v_][:, D:2 * D], func=AF.Copy,
                                                            scale=wgt[sv][:, hk:hk + 1]),
                              reads=[r_uv[v_], r_wgt[sv][pgrp]], writes=[r_vsb[b_]])
                        for dmb in range(4):
                            kb.op("pe", lambda e: e.matmul(PS[dmb][:], lhsT=ident_b[:],
                                                           rhs=vsb[b_][:, dmb * 512:(dmb + 1) * 512],
                                                           start=(hk == 0), stop=(hk == 127)),
                                  reads=[r_vsb[b_], r_const], writes=[PR[dmb]])
                    if pgrp == 128 // GP - 1:
                        for dmb in range(4):
                            kb.op("dve", lambda e: e.tensor_tensor(out=yt[sv][:, dmb * 512:(dmb + 1) * 512],
                                                                   in0=PS[dmb][:],
                                                                   in1=hE[sv][:, dmb * 512:(dmb + 1) * 512], op=ALU.add),
                                  reads=[PR[dmb], r_hE[sv]], writes=[r_yt[sv]])
                        kb.dma("sp", r_yt[sv], lambda e: e.dma_start(out=y[ptt * 128:(ptt + 1) * 128, :],
                                                                     in_=yt[sv][:]),
                               reads=[r_yt[sv]], writes=[r_y])
                prev = step
            kb.wait_all("sp", [r_y] + r_yt)
    nc._dump_list = dump_list
    return nc


_NAMES = ["x", "norm1_w", "w_in", "hg_lb_logits", "hg_norm_w", "q_norm_w", "kc_norm_w", "ks_norm_w", "kw_norm_w",
          "cmp_pos_k", "cmp_pos_v", "w_ck1", "w_ck2", "w_cv1", "w_cv2", "w_out", "norm2_w", "peer_w_q",
          "peer_sub_keys", "peer_u", "peer_v"]


def make_in_maps(inputs):
    tb = host_tables()
    shared = {}
    for k in _NAMES:
        if k == "x":
            continue
        shared[k] = np.ascontiguousarray(np.asarray(inputs[k], dtype=np.float32))
    for k, v in tb.items():
        shared["tb_" + k] = np.ascontiguousarray(v)
    xs = np.asarray(inputs["x"], dtype=np.float32)
    maps = []
    for b in range(8):
        m = dict(shared)
        m["x"] = np.ascontiguousarray(xs[b])
        maps.append(m)
    return maps


def kernel(**inputs):
    nc = build(debug=False)
    in_maps = make_in_maps(inputs)
    res = run_bass_kernel_spmd(nc, in_maps, core_ids=list(range(8)))
    out = np.stack([np.asarray(r["y"], dtype=np.float32) for r in res.results], axis=0)
    return out
```
